# Optimizing a Trainium2 kernel written in Bass

```python
import math
import jax
import jax.numpy as jnp
from jax import lax
import numpy as np

D_MODEL = 1024
BATCH = 32
SEQ = 2048
DEPTH = 2

CTX_LEN = 256
GRID_W = 64
RMS_EPS = 1e-6
L2_EPS = 1e-6
A_WIDTH = D_MODEL // 2
GDN_HEADS = 8
GDN_DK = (D_MODEL // 2) // GDN_HEADS
GDN_DV = (D_MODEL // 2) // GDN_HEADS
GDN_CHUNK = 64
QKV_WIDTH = 2 * GDN_HEADS * GDN_DK + GDN_HEADS * GDN_DV
EVEN_SPLITS = (A_WIDTH, A_WIDTH, A_WIDTH, QKV_WIDTH, GDN_HEADS * GDN_DV, GDN_HEADS, GDN_HEADS, GDN_HEADS, GDN_HEADS)
EVEN_IN = sum(EVEN_SPLITS)
EVEN_OUT = A_WIDTH + GDN_HEADS * GDN_DV
ATTN_HEADS = 8
ATTN_KV_HEADS = 2
ATTN_GROUP = ATTN_HEADS // ATTN_KV_HEADS
ATTN_HEAD_DIM = D_MODEL // ATTN_HEADS
ODD_IN = (ATTN_HEADS + 2 * ATTN_KV_HEADS) * ATTN_HEAD_DIM
Q_BLOCK = 128
ROPE_THETA = 10000.0
N_GROUPS = 8
EXPERTS_PER_GROUP = 8
N_EXPERTS = N_GROUPS * EXPERTS_PER_GROUP
TOP_K = 2
D_EXPERT = (3 * D_MODEL) // 8
MOE_BLOCK = 256

kernel_name = 'hybrid_conv_deltanet_gqa_hier_moe_dit'

F32 = jnp.float32


def _split(t, sizes):
    idx = [int(s) for s in np.cumsum(sizes)[:-1]]
    return jnp.split(t, idx, axis=-1)


def _rmsnorm(x, g):
    xf = x.astype(F32)
    return (xf * lax.rsqrt(jnp.mean(xf * xf, axis=-1, keepdims=True) + RMS_EPS)).astype(x.dtype) * g


def _l2norm(x):
    xf = x.astype(F32)
    return (xf * lax.rsqrt(jnp.sum(xf * xf, axis=-1, keepdims=True) + L2_EPS)).astype(x.dtype)


def _dwconv3(x, w):
    ch = x.shape[-1]
    return lax.conv_general_dilated(x, w[:, None, :].astype(x.dtype), window_strides=(1,), padding=((1, 1),),
                                    dimension_numbers=('NWC', 'WIO', 'NWC'), feature_group_count=ch)


def _axial_rope(rows, cols):
    n_freq = ATTN_HEAD_DIM // 4
    inv = ROPE_THETA ** (-jnp.arange(n_freq, dtype=F32) / n_freq)
    ang = jnp.concatenate([rows.astype(F32)[:, None] * inv, cols.astype(F32)[:, None] * inv], axis=-1)
    return jnp.cos(ang), jnp.sin(ang)


def _rope(x, cos, sin):
    xf = x.astype(F32).reshape(*x.shape[:-1], -1, 2)
    x1, x2 = xf[..., 0], xf[..., 1]
    out = jnp.stack([x1 * cos - x2 * sin, x1 * sin + x2 * cos], axis=-1)
    return out.reshape(x.shape).astype(x.dtype)


def _gdn_chunked(q, k, v, g, beta, s0):
    b, h, l, dk = q.shape
    dv = v.shape[-1]
    c = GDN_CHUNK
    n = l // c
    q, k, v = (t.astype(F32).reshape(b, h, n, c, -1) for t in (q, k, v))
    g = g.astype(F32).reshape(b, h, n, c)
    beta = beta.astype(F32).reshape(b, h, n, c)
    gc = jnp.cumsum(g, axis=-1)
    incl = jnp.tril(jnp.ones((c, c), dtype=bool))
    strict = jnp.tril(jnp.ones((c, c), dtype=bool), -1)
    decay = jnp.exp(jnp.where(incl, gc[..., :, None] - gc[..., None, :], -jnp.inf))
    kb = k * beta[..., None]
    a_mat = jnp.eye(c, dtype=F32) + jnp.where(strict, jnp.einsum('bhnid,bhnjd->bhnij', kb, k) * decay, 0.0)
    rhs = jnp.concatenate([v * beta[..., None], kb * jnp.exp(gc)[..., None]], axis=-1)
    sol = lax.linalg.triangular_solve(a_mat, rhs, left_side=True, lower=True, unit_diagonal=True)
    u, w = sol[..., :dv], sol[..., dv:]
    attn = jnp.einsum('bhnid,bhnjd->bhnij', q, k) * decay
    qg = q * jnp.exp(gc)[..., None]
    kg = k * jnp.exp(gc[..., -1:] - gc)[..., None]
    g_last = jnp.exp(gc[..., -1])
    xs = tuple(jnp.moveaxis(t, 2, 0) for t in (u, w, attn, qg, kg, g_last))

    def step(s, inp):
        u_n, w_n, a_n, qg_n, kg_n, gl_n = inp
        v_new = u_n - jnp.einsum('bhcd,bhde->bhce', w_n, s)
        o_n = jnp.einsum('bhcd,bhde->bhce', qg_n, s) + jnp.einsum('bhij,bhje->bhie', a_n, v_new)
        s = s * gl_n[..., None, None] + jnp.einsum('bhcd,bhce->bhde', kg_n, v_new)
        return s, o_n

    s_fin, o = lax.scan(step, s0.astype(F32), xs)
    return s_fin, jnp.moveaxis(o, 0, 2).reshape(b, h, l, dv)


def _gdn_direction(q, k, v, g, beta, s0, reverse):
    if reverse:
        q, k, v, g, beta = (jnp.flip(t, axis=2) for t in (q, k, v, g, beta))
    s_fin, o = _gdn_chunked(q, k, v, g, beta, s0)
    if reverse:
        o = jnp.flip(o, axis=2)
    return s_fin, o


def _gdn_gates(a_in, b_in, a_log, dt_bias):
    g = -jnp.exp(a_log.astype(F32)) * jax.nn.softplus(a_in.astype(F32) + dt_bias.astype(F32))
    beta = jax.nn.sigmoid(b_in.astype(F32))
    return jnp.swapaxes(g, 1, 2), jnp.swapaxes(beta, 1, 2)


def _conv_deltanet_mixer(h_l, h_c, w_in, conv_a, conv_qkv, a_log, dt_bias, gnorm, w_out, need_ctx):
    def prep(h):
        b, l, _ = h.shape
        xa, gb, gcv, qkv, z, af, bf, ab, bb = _split(h @ w_in, EVEN_SPLITS)
        qkv = jax.nn.silu(_dwconv3(qkv, conv_qkv))
        q, k, v = _split(qkv, (GDN_HEADS * GDN_DK, GDN_HEADS * GDN_DK, GDN_HEADS * GDN_DV))
        heads = lambda t: jnp.swapaxes(t.reshape(b, l, GDN_HEADS, -1), 1, 2)
        q = _l2norm(heads(q)) * (GDN_DK ** -0.5)
        k = _l2norm(heads(k))
        v = heads(v)
        gates = (_gdn_gates(af, bf, a_log[0], dt_bias[0]), _gdn_gates(ab, bb, a_log[1], dt_bias[1]))
        return (xa, gb, gcv), z, (q, k, v), gates

    a_l, z_l, qkv_l, gates_l = prep(h_l)
    a_c, z_c, qkv_c, gates_c = prep(h_c)
    s0 = jnp.zeros((h_l.shape[0], GDN_HEADS, GDN_DK, GDN_DV), F32)
    outs_l, outs_c = [], []
    for d in range(2):
        s_ctx, o_cd = _gdn_direction(*qkv_c, *gates_c[d], s0, d == 1)
        _, o_ld = _gdn_direction(*qkv_l, *gates_l[d], s_ctx, d == 1)
        outs_l.append(o_ld)
        outs_c.append(o_cd)

    def merge(a_parts, z, o):
        xa, gb, gcv = a_parts
        y_a = gb * _dwconv3(gcv * xa, conv_a)
        bsz, l, _ = z.shape
        o = jnp.swapaxes(o, 1, 2)
        y_b = _rmsnorm(o, gnorm) * jax.nn.silu(z.astype(F32)).reshape(bsz, l, GDN_HEADS, GDN_DV)
        y_b = y_b.reshape(bsz, l, GDN_HEADS * GDN_DV).astype(y_a.dtype)
        return jnp.concatenate([y_a, y_b], axis=-1) @ w_out

    y_l = merge(a_l, z_l, outs_l[0] + outs_l[1])
    y_c = merge(a_c, z_c, outs_c[0] + outs_c[1]) if need_ctx else None
    return y_l, y_c


def _attend(q, k, v):
    s = jnp.einsum('bqkgd,bskd->bkgqs', q, k, preferred_element_type=F32) * (ATTN_HEAD_DIM ** -0.5)
    p = jax.nn.softmax(s, axis=-1).astype(v.dtype)
    return jnp.einsum('bkgqs,bskd->bqkgd', p, v)


def _attention_mixer(h_l, h_c, w_qkv, q_norm, k_norm, w_o, cos, sin, need_ctx):
    def heads(h):
        b, l, _ = h.shape
        q, k, v = _split(h @ w_qkv, (ATTN_HEADS * ATTN_HEAD_DIM, ATTN_KV_HEADS * ATTN_HEAD_DIM, ATTN_KV_HEADS * ATTN_HEAD_DIM))
        q = _rmsnorm(q.reshape(b, l, ATTN_KV_HEADS, ATTN_GROUP, ATTN_HEAD_DIM), q_norm)
        k = _rmsnorm(k.reshape(b, l, ATTN_KV_HEADS, ATTN_HEAD_DIM), k_norm)
        v = v.reshape(b, l, ATTN_KV_HEADS, ATTN_HEAD_DIM)
        return q, k, v

    b, l, _ = h_l.shape
    q_l, k_l, v_l = heads(h_l)
    q_l = _rope(q_l, cos[:, None, None, :], sin[:, None, None, :])
    k_l = _rope(k_l, cos[:, None, :], sin[:, None, :])
    q_c, k_c, v_c = heads(h_c)
    k_all = jnp.concatenate([k_l, k_c], axis=1)
    v_all = jnp.concatenate([v_l, v_c], axis=1)
    nb = l // Q_BLOCK
    q_blocks = jnp.moveaxis(q_l.reshape(b, nb, Q_BLOCK, ATTN_KV_HEADS, ATTN_GROUP, ATTN_HEAD_DIM), 1, 0)
    o_l = lax.map(lambda qb: _attend(qb, k_all, v_all), q_blocks)
    y_l = jnp.moveaxis(o_l, 0, 1).reshape(b, l, ATTN_HEADS * ATTN_HEAD_DIM) @ w_o
    y_c = None
    if need_ctx:
        y_c = _attend(q_c, k_c, v_c).reshape(b, h_c.shape[1], ATTN_HEADS * ATTN_HEAD_DIM) @ w_o
    return y_l, y_c


def _hier_moe(h, w_group, w_expert, w_gate, w_up, w_down):
    t, d = h.shape
    g_logits = (h @ w_group).astype(F32)
    grp = jnp.argmax(g_logits, axis=-1).astype(jnp.int32)
    g_prob = jnp.take_along_axis(jax.nn.softmax(g_logits, axis=-1), grp[:, None], axis=1)
    e_logits = (h @ w_expert).astype(F32).reshape(t, N_GROUPS, EXPERTS_PER_GROUP)
    e_in = jnp.take_along_axis(e_logits, grp[:, None, None], axis=1)[:, 0]
    top_val, top_idx = lax.top_k(e_in, TOP_K)
    gate = jax.nn.softmax(top_val, axis=-1) * g_prob
    expert = grp[:, None] * EXPERTS_PER_GROUP + top_idx.astype(jnp.int32)
    tk = t * TOP_K
    flat_e = expert.reshape(tk)
    order = jnp.argsort(flat_e).astype(jnp.int32)
    se = flat_e[order]
    stok = order // TOP_K
    sw = gate.reshape(tk)[order]
    counts = jnp.zeros((N_EXPERTS,), jnp.int32).at[flat_e].add(1)
    starts = jnp.cumsum(counts) - counts
    padded = (counts + MOE_BLOCK - 1) // MOE_BLOCK * MOE_BLOCK
    pad_starts = jnp.cumsum(padded) - padded
    dest = pad_starts[se] + jnp.arange(tk, dtype=jnp.int32) - starts[se]
    n_blocks = -(-tk // MOE_BLOCK) + N_EXPERTS
    p = n_blocks * MOE_BLOCK
    src = jnp.full((p,), t, jnp.int32).at[dest].set(stok)
    wbuf = jnp.zeros((p,), h.dtype).at[dest].set(sw.astype(h.dtype))
    buf = jnp.concatenate([h, jnp.zeros((1, d), h.dtype)], axis=0)[src].reshape(n_blocks, MOE_BLOCK, d)
    blk_start = jnp.arange(n_blocks, dtype=jnp.int32) * MOE_BLOCK
    blk_expert = jnp.minimum(jnp.searchsorted(pad_starts + padded, blk_start, side='right'), N_EXPERTS - 1)

    def expert_block(args):
        xb, e = args
        return (jax.nn.silu(xb @ w_gate[e]) * (xb @ w_up[e])) @ w_down[e]

    ybuf = lax.map(expert_block, (buf, blk_expert)).reshape(p, d)
    return jax.ops.segment_sum(ybuf * wbuf[:, None], src, num_segments=t + 1)[:t]


def setup_inputs(seed: int = 0) -> dict:
    key = jax.random.key(seed)
    ks = iter(jax.random.split(key, 40))
    nrm = lambda shape, scale: jax.random.normal(next(ks), shape, jnp.float32) * scale
    d = D_MODEL
    ne = (DEPTH + 1) // 2
    no = DEPTH // 2
    x = nrm((BATCH, SEQ, d), 1.0)
    c = nrm((BATCH, d), 1.0)
    ctx = nrm((BATCH, CTX_LEN, d), 1.0)
    c_ctx = nrm((d,), 1.0)
    mod_w = nrm((DEPTH, d, 6 * d), 0.5 * d ** -0.5)
    mod_b = nrm((DEPTH, 6 * d), 0.02)
    norm1 = 1.0 + nrm((DEPTH, d), 0.02)
    norm2 = 1.0 + nrm((DEPTH, d), 0.02)
    ab_w_in = nrm((ne, d, EVEN_IN), d ** -0.5)
    ab_conv_a = nrm((ne, 3, A_WIDTH), 3 ** -0.5)
    ab_conv_qkv = nrm((ne, 3, QKV_WIDTH), 3 ** -0.5)
    ab_a_log = jnp.log(jax.random.uniform(next(ks), (ne, 2, GDN_HEADS), jnp.float32, 1.0, 16.0))
    dt = jnp.exp(jax.random.uniform(next(ks), (ne, 2, GDN_HEADS), jnp.float32, math.log(1e-3), math.log(1e-1)))
    ab_dt_bias = dt + jnp.log(-jnp.expm1(-dt))
    ab_gnorm = 1.0 + nrm((ne, GDN_DV), 0.02)
    ab_w_out = nrm((ne, EVEN_OUT, d), EVEN_OUT ** -0.5)
    attn_w_qkv = nrm((no, d, ODD_IN), d ** -0.5)
    attn_q_norm = 1.0 + nrm((no, ATTN_HEAD_DIM), 0.02)
    attn_k_norm = 1.0 + nrm((no, ATTN_HEAD_DIM), 0.02)
    attn_w_o = nrm((no, ATTN_HEADS * ATTN_HEAD_DIM, d), (ATTN_HEADS * ATTN_HEAD_DIM) ** -0.5)
    moe_w_group = nrm((DEPTH, d, N_GROUPS), d ** -0.5)
    moe_w_expert = nrm((DEPTH, d, N_EXPERTS), d ** -0.5)
    moe_w_gate = nrm((DEPTH, N_EXPERTS, d, D_EXPERT), d ** -0.5)
    moe_w_up = nrm((DEPTH, N_EXPERTS, d, D_EXPERT), d ** -0.5)
    moe_w_down = nrm((DEPTH, N_EXPERTS, D_EXPERT, d), D_EXPERT ** -0.5)
    final_norm = 1.0 + nrm((d,), 0.02)
    return {'x': x, 'c': c, 'ctx': ctx, 'c_ctx': c_ctx, 'mod_w': mod_w, 'mod_b': mod_b,
            'norm1': norm1, 'norm2': norm2, 'ab_w_in': ab_w_in, 'ab_conv_a': ab_conv_a,
            'ab_conv_qkv': ab_conv_qkv, 'ab_a_log': ab_a_log, 'ab_dt_bias': ab_dt_bias,
            'ab_gnorm': ab_gnorm, 'ab_w_out': ab_w_out, 'attn_w_qkv': attn_w_qkv,
            'attn_q_norm': attn_q_norm, 'attn_k_norm': attn_k_norm, 'attn_w_o': attn_w_o,
            'moe_w_group': moe_w_group, 'moe_w_expert': moe_w_expert, 'moe_w_gate': moe_w_gate,
            'moe_w_up': moe_w_up, 'moe_w_down': moe_w_down, 'final_norm': final_norm}


def reference(x, c, ctx, c_ctx, mod_w, mod_b, norm1, norm2, ab_w_in, ab_conv_a, ab_conv_qkv,
              ab_a_log, ab_dt_bias, ab_gnorm, ab_w_out, attn_w_qkv, attn_q_norm, attn_k_norm,
              attn_w_o, moe_w_group, moe_w_expert, moe_w_gate, moe_w_up, moe_w_down, final_norm):
    b, l, d = x.shape
    n_ctx = ctx.shape[1]
    ROWS = l // GRID_W
    rows = jnp.repeat(jnp.arange(ROWS, dtype=jnp.int32), GRID_W)
    cols = jnp.tile(jnp.arange(GRID_W, dtype=jnp.int32), ROWS)
    cos, sin = _axial_rope(rows, cols)
    cond_l = jax.nn.silu(c)
    cond_c = jax.nn.silu(c_ctx)
    for i in range(DEPTH):
        need_ctx = i < DEPTH - 1
        j = i // 2
        mod_l = (cond_l @ mod_w[i] + mod_b[i])[:, None, :]
        mod_c = cond_c @ mod_w[i] + mod_b[i]
        sh1, sc1, g1, sh2, sc2, g2 = jnp.split(mod_l, 6, axis=-1)
        csh1, csc1, cg1, csh2, csc2, cg2 = jnp.split(mod_c, 6, axis=-1)
        h_l = _rmsnorm(x, norm1[i]) * (1.0 + sc1) + sh1
        h_c = _rmsnorm(ctx, norm1[i]) * (1.0 + csc1) + csh1
        if i % 2 == 0:
            y_l, y_c = _conv_deltanet_mixer(h_l, h_c, ab_w_in[j], ab_conv_a[j], ab_conv_qkv[j], ab_a_log[j],
                                            ab_dt_bias[j], ab_gnorm[j], ab_w_out[j], need_ctx)
        else:
            y_l, y_c = _attention_mixer(h_l, h_c, attn_w_qkv[j], attn_q_norm[j], attn_k_norm[j], attn_w_o[j],
                                        cos, sin, need_ctx)
        x = x + g1 * y_l
        h2_l = (_rmsnorm(x, norm2[i]) * (1.0 + sc2) + sh2).reshape(b * l, d)
        if need_ctx:
            ctx = ctx + cg1 * y_c
            h2_c = (_rmsnorm(ctx, norm2[i]) * (1.0 + csc2) + csh2).reshape(b * n_ctx, d)
            tokens = jnp.concatenate([h2_l, h2_c], axis=0)
        else:
            tokens = h2_l
        y = _hier_moe(tokens, moe_w_group[i], moe_w_expert[i], moe_w_gate[i], moe_w_up[i], moe_w_down[i])
        x = x + g2 * y[:b * l].reshape(b, l, d)
        if need_ctx:
            ctx = ctx + cg2 * y[b * l:].reshape(b, n_ctx, d)
    return _rmsnorm(x, final_norm)
```

```python
from contextlib import ExitStack
import numpy as np
import concourse.bass as bass
import concourse.mybir as mybir
from concourse.bass_utils import run_bass_kernel_spmd

F32 = mybir.dt.float32
F32R = mybir.dt.float32r
I32 = mybir.dt.int32
U32 = mybir.dt.uint32
AF = mybir.ActivationFunctionType
ALU = mybir.AluOpType
AX = mybir.AxisListType

N_DMA_SEMS = 24


class T:
    _n = 0

    def __init__(self, ap, name=None, psum=False):
        self.ap = ap
        self.psum = psum
        T._n += 1
        self.key = (name or "t") + str(T._n)

    def __getitem__(self, idx):
        return self.ap[idx]


class Prog:
    ENG = ("pe", "act", "dve", "pool", "sp")

    def __init__(self, nc, stack):
        self.nc = nc
        self.esem = {e: stack.enter_context(nc.semaphore("s_" + e)) for e in ("pe", "act", "dve", "pool")}
        self.ecount = {e: 0 for e in self.esem}
        self.dsems = [stack.enter_context(nc.semaphore("s_d%d" % i)) for i in range(N_DMA_SEMS)]
        self.swsems = [stack.enter_context(nc.semaphore("s_sw%d" % i)) for i in range(48)]
        self.sw_used = 0
        self.sw_dirty = []
        self.dummy = stack.enter_context(nc.sbuf_tensor("dummy_sw", [1, 8], F32))
        self.dcount = [0] * N_DMA_SEMS
        self.dnext = 0
        self.waited = {e: {} for e in self.ENG}
        self.lastw = {}
        self.readers = {}
        self.ops = {e: [] for e in self.ENG}
        self.n_inst = 0

    def _sem(self, sk):
        return self.esem[sk] if isinstance(sk, str) else self.dsems[sk[1]]

    def _collect(self, eng, reads, writes, extra=()):
        need = {}

        def add(tok):
            sk, v = tok
            if sk == "pe" and eng == "pe":
                return
            if need.get(sk, 0) < v:
                need[sk] = v

        for t in reads:
            k = t.key
            if k in self.lastw:
                add(self.lastw[k])
        for t in writes:
            k = t.key
            if k in self.lastw:
                add(self.lastw[k])
            for sk, v in self.readers.get(k, {}).items():
                add((sk, v))
        for tok in extra:
            add(tok)
        waits = []
        w = self.waited[eng]
        for sk, v in need.items():
            if w.get(sk, 0) < v:
                w[sk] = v
                waits.append((sk, v))
        return waits

    def _commit(self, tok, reads, writes):
        sk, v = tok
        for t in reads:
            r = self.readers.setdefault(t.key, {})
            if r.get(sk, 0) < v:
                r[sk] = v
        for t in writes:
            self.lastw[t.key] = tok
            self.readers[t.key] = {}

    limit = 10 ** 9
    log = None
    sw_count_mode = True
    sw_total = 0

    def _skip(self):
        if self.n_inst >= Prog.limit:
            return True
        if Prog.log is not None:
            import sys
            f = sys._getframe(2)
            Prog.log.append((self.n_inst, f.f_lineno, f.f_back.f_lineno))
        return False

    def op(self, eng, fn, r=(), w=()):
        if self._skip():
            return None
        pr = [t for t in r if t.psum]
        if pr:
            r = [t for t in r if not t.psum]
            w = list(w) + [t for t in pr if t not in w]
        waits = self._collect(eng, r, w)
        self.ecount[eng] += 1
        tok = (eng, self.ecount[eng])
        self.ops[eng].append((waits, fn, eng, 1))
        self._commit(tok, r, w)
        self.n_inst += 1
        return tok

    def dma(self, fn, r=(), w=(), q="sp"):
        if self._skip():
            return None
        if q == "pool":
            if self.sw_used == len(self.swsems):
                self.flush("sw")
            j = self.sw_used
            self.sw_used += 1
            waits = self._collect("pool", r, w)
            self.ecount["pool"] += 1
            tok = ("pool", self.ecount["pool"])
            self.ops["pool"].append((waits, fn, "swdma", j))
            self._commit(tok, r, w)
            self.n_inst += 1
            return tok
        i = self.dnext
        self.dnext = (self.dnext + 1) % N_DMA_SEMS
        extra = []
        if self.dcount[i] > 0:
            extra.append((("d", i), self.dcount[i]))
        waits = self._collect(q, r, w, extra)
        self.dcount[i] += 16
        tok = (("d", i), self.dcount[i])
        self.ops[q].append((waits, fn, ("d", i), 16))
        self._commit(tok, r, w)
        self.n_inst += 1
        return tok

    def flush(self, name="blk"):
        nc = self.nc
        final = [(e, self.ecount[e]) for e in self.esem if self.ecount[e] > 0]
        final += [(("d", i), c) for i, c in enumerate(self.dcount) if c > 0]
        tail = {}
        for e in self.ENG:
            ws = []
            for sk, v in final:
                if sk == "pe" and e == "pe":
                    continue
                if self.waited[e].get(sk, 0) < v:
                    self.waited[e][sk] = v
                    ws.append((sk, v))
            tail[e] = ws
        ops = self.ops
        self.ops = {e: [] for e in self.ENG}
        semf = self._sem

        swsems, dummy = self.swsems, self.dummy
        dirty = self.sw_dirty
        self.sw_dirty = list(range(self.sw_used))
        self.sw_used = 0

        def run(engine, lst, tl, clear=False):
            if clear and not Prog.sw_count_mode:
                for j in dirty:
                    engine.sem_clear(swsems[j])
            for waits, fn, sk, inc in lst:
                for wk, v in waits:
                    engine.wait_ge(semf(wk), v)
                if sk == "swdma":
                    if Prog.sw_count_mode:
                        self.sw_total += 16
                        fn(engine).then_inc(swsems[0], 16)
                        engine.wait_ge(swsems[0], self.sw_total)
                    else:
                        fn(engine).then_inc(swsems[inc], 16)
                        engine.wait_ge(swsems[inc], 16)
                    engine.memset(dummy[0:1, 0:1], 0.0).then_inc(semf("pool"), 1)
                    continue
                fn(engine).then_inc(semf(sk), inc)
            for wk, v in tl:
                engine.wait_ge(semf(wk), v)

        with nc.Block() as block:
            @block.tensor
            def _(e):
                run(e, ops["pe"], tail["pe"])

            @block.scalar
            def _(e):
                run(e, ops["act"], tail["act"])

            @block.vector
            def _(e):
                run(e, ops["dve"], tail["dve"])

            @block.gpsimd
            def _(e):
                run(e, ops["pool"], tail["pool"], clear=True)

            @block.sync
            def _(e):
                run(e, ops["sp"], tail["sp"])
        self.lastw = {}
        self.readers = {}


class Ctx:
    def __init__(self, nc, stack):
        self.nc = nc
        self.stack = stack

    def sb(self, shape, dt=F32, name=None):
        T._n += 1
        nm = (name or "sb") + "_%d" % T._n
        return T(self.stack.enter_context(self.nc.sbuf_tensor(nm, list(shape), dt)), nm)

    def ps(self, shape=(128, 512), dt=F32, name=None):
        T._n += 1
        nm = (name or "ps") + "_%d" % T._n
        return T(self.stack.enter_context(self.nc.psum_tensor(nm, list(shape), dt)), nm, psum=True)


def r32(ap):
    return ap.bitcast(F32R)


def emit_rstd(P, C, xt, rows, d, eps, junk):
    ss = C.sb([128, 1], name="ss")
    rs = C.sb([128, 1], name="rs")
    P.op("act", lambda e: e.activation(out=junk[:rows, :d], in_=xt[:rows, :d], func=AF.Square,
                                       accum_out=ss[:rows, :]), r=[xt], w=[junk, ss])
    P.op("dve", lambda e: e.tensor_scalar(out=ss[:rows, :], in0=ss[:rows, :], scalar1=1.0 / d, scalar2=eps,
                                          op0=ALU.mult, op1=ALU.add), r=[ss], w=[ss])
    P.op("act", lambda e: e.activation(out=ss[:rows, :], in_=ss[:rows, :], func=AF.Sqrt), r=[ss], w=[ss])
    P.op("dve", lambda e: e.reciprocal(out=rs[:rows, :], in_=ss[:rows, :]), r=[ss], w=[rs])
    return rs


def phase_final_norm(nc, P, x_d, gain_d, out_d, ntok, d=1024, eps=1e-6):
    with ExitStack() as st:
        C = Ctx(nc, st)
        g = C.sb([128, d], name="gain")
        junk = C.sb([128, d], name="junk")
        P.dma(lambda e: e.dma_start(out=g[:, :], in_=gain_d.ap.partition_broadcast(128)), r=[gain_d], w=[g])
        xts = [C.sb([128, d], name="xt") for _ in range(2)]
        for i in range(ntok // 128):
            xt = xts[i % 2]
            P.dma(lambda e, xt=xt, i=i: e.dma_start(out=xt[:, :], in_=x_d[i * 128:(i + 1) * 128, :]),
                  r=[x_d], w=[xt])
            rs = emit_rstd(P, C, xt, 128, d, eps, junk)
            P.op("dve", lambda e, xt=xt, rs=rs: e.scalar_tensor_tensor(
                out=xt[:, :], in0=xt[:, :], scalar=rs[:, 0:1], in1=g[:, :], op0=ALU.mult, op1=ALU.mult),
                r=[xt, rs, g], w=[xt])
            P.dma(lambda e, xt=xt, i=i: e.dma_start(out=out_d[i * 128:(i + 1) * 128, :], in_=xt[:, :]),
                  r=[xt], w=[out_d])
        P.flush("fnorm")


class Cfg:
    def __init__(self, NB=4, L=2048, NCTX=256, GRID_W=64, CAP=512):
        self.NB, self.L, self.NCTX, self.GRID_W, self.CAP = NB, L, NCTX, GRID_W, CAP
        self.TB = L + NCTX
        self.D = 1024
        self.NE = 64
        self.DE = 384


NEG = -30000.0
C_ID = 0
C_BO = 128
C_ONES = 256
C_MINC0 = 384
C_MINC1 = 448
C_MST0 = 512
C_MST1 = 576
C_SEL = 640
C_UT = 640 + 1024
C_IOTA = C_UT + 128
C_END = C_IOTA + 64


def build_consts():
    c = np.zeros((128, C_END), np.float32)
    c[:, C_ID:C_ID + 128] = np.eye(128)
    c[:64, C_BO:C_BO + 64] = 1
    c[64:, C_BO + 64:C_BO + 128] = 1
    c[:, C_ONES:C_ONES + 128] = 1
    i = np.arange(64)[:, None]
    j = np.arange(64)[None, :]
    c[:64, C_MINC0:C_MINC0 + 64] = np.where(j <= i, 0, NEG)
    c[:64, C_MINC1:C_MINC1 + 64] = np.where(j >= i, 0, NEG)
    c[:64, C_MST0:C_MST0 + 64] = (j < i)
    c[:64, C_MST1:C_MST1 + 64] = (j > i)
    sel = np.zeros((16, 16, 64), np.float32)
    for h in range(16):
        sel[h, h, :] = 1
    c[:16, C_SEL:C_SEL + 1024] = sel.reshape(16, 1024)
    k = np.arange(128)[:, None]
    m = np.arange(128)[None, :]
    c[:, C_UT:C_UT + 128] = (k < m)
    c[:, C_IOTA:C_IOTA + 64] = np.arange(64)[None, :]
    return c


def load_consts(P, C, cst_d):
    cst = C.sb([128, C_END], name="cst")
    P.dma(lambda e: e.dma_start(out=cst[:, :], in_=cst_d[:, :]), r=[cst_d], w=[cst])
    return cst


def transpose_tile(P, src, src_cols, rows, dst, dst_fn, cst, pss, ev, rnd=False):
    c0, ncol = src_cols
    nch = ncol // 128
    g = 0
    for s in range(0, nch, 4):
        n = min(4, nch - s)
        ps = pss[ev[0] % len(pss)]
        for q in range(n):
            P.op("pe", lambda e, ps=ps, q=q, s=s: e.transpose(
                out=ps[:, q * 128:q * 128 + rows], in_=src[:rows, c0 + (s + q) * 128:c0 + (s + q + 1) * 128],
                identity=cst[:rows, C_ID:C_ID + rows]), r=[src, cst], w=[ps])
        eng = "act" if ev[0] % 2 == 0 else "dve"
        o = dst_fn(g, n)
        if rnd:
            o = r32(o)
        src_ap = ps[:, :n * 128].rearrange("p (n t) -> p n t", t=128)[:, :, :rows]
        if eng == "act":
            P.op("act", lambda e, o=o, a=src_ap: e.copy(out=o, in_=a), r=[ps], w=[dst])
        else:
            P.op("dve", lambda e, o=o, a=src_ap: e.tensor_copy(out=o, in_=a), r=[ps], w=[dst])
        ev[0] += 1
        g += 1


def phase_mod(nc, P, cT_d, modw_d, modb_d, MOD_d, NBc):
    with ExitStack() as st:
        C = Ctx(nc, st)
        cT = C.sb([128, 8, NBc], name="cT")
        P.dma(lambda e: e.dma_start(out=cT[:, :, :], in_=cT_d.ap.rearrange("(k p) b -> p k b", p=128)),
              r=[cT_d], w=[cT])
        P.op("act", lambda e: e.activation(out=cT[:, :, :], in_=cT[:, :, :], func=AF.Silu), r=[cT], w=[cT])
        ws = [C.sb([128, 8, 512], name="mw") for _ in range(2)]
        pss = [C.ps(name="mps") for _ in range(2)]
        for layer in range(2):
            bias = C.sb([NBc, 6144], name="mb")
            res = C.sb([NBc, 6144], name="mres")
            P.dma(lambda e, bias=bias, layer=layer: e.dma_start(
                out=bias[:, :], in_=modb_d[layer, :].partition_broadcast(NBc)), r=[modb_d], w=[bias])
            for cb in range(12):
                w = ws[cb % 2]
                ps = pss[cb % 2]
                P.dma(lambda e, w=w, layer=layer, cb=cb: e.dma_start(
                    out=w[:, :, :], in_=modw_d[layer, :, cb * 512:(cb + 1) * 512].rearrange("(k p) n -> p k n", p=128)),
                    r=[modw_d], w=[w])
                for k in range(8):
                    P.op("pe", lambda e, ps=ps, w=w, k=k: e.matmul(
                        ps[:NBc, :], lhsT=cT[:, k, :], rhs=w[:, k, :], start=(k == 0), stop=(k == 7)),
                        r=[cT, w], w=[ps])
                P.op("dve", lambda e, ps=ps, res=res, bias=bias, cb=cb: e.tensor_tensor(
                    out=res[:, cb * 512:(cb + 1) * 512], in0=ps[:NBc, :], in1=bias[:, cb * 512:(cb + 1) * 512],
                    op=ALU.add), r=[ps, bias], w=[res])
            P.dma(lambda e, res=res, layer=layer: e.dma_start(out=MOD_d[layer, :, :], in_=res[:, :]),
                  r=[res], w=[MOD_d])
        P.flush("mod")


def load_mod_rows(P, C, MOD_d, layer, b, k, gain_d=None, plus1=False):
    t = C.sb([128, 1024], name="modrow")
    P.dma(lambda e: e.dma_start(out=t[:, :], in_=MOD_d[layer, b, k * 1024:(k + 1) * 1024].partition_broadcast(128)),
          r=[MOD_d], w=[t])
    if plus1:
        g = C.sb([128, 1024], name="gainrow")
        P.dma(lambda e: e.dma_start(out=g[:, :], in_=gain_d.ap.partition_broadcast(128)), r=[gain_d], w=[g])
        P.op("dve", lambda e: e.scalar_tensor_tensor(out=t[:, :], in0=t[:, :], scalar=1.0, in1=g[:, :],
                                                      op0=ALU.add, op1=ALU.mult), r=[t, g], w=[t])
    return t


def norm_mod_tile(P, C, xt, rows, A, SH, h, eps=1e-6):
    rs = emit_rstd(P, C, xt, rows, 1024, eps, h)
    P.op("dve", lambda e: e.scalar_tensor_tensor(out=h[:rows, :], in0=xt[:rows, :], scalar=rs[:rows, 0:1],
                                                  in1=A[:rows, :], op0=ALU.mult, op1=ALU.mult),
         r=[xt, rs, A], w=[h])
    P.op("dve", lambda e: e.tensor_tensor(out=h[:rows, :], in0=h[:rows, :], in1=SH[:rows, :], op=ALU.add),
         r=[h, SH], w=[h])


WIN_COLS = 3072 + 576


def token_blocks(cfg):
    out = []
    for s in range(0, cfg.L, 512):
        out.append((s, min(512, cfg.L - s), False))
    for s in range(0, cfg.NCTX, 512):
        out.append((cfg.L + s, min(512, cfg.NCTX - s), True))
    return out


def phase_proj0(nc, P, cfg, XR_d, MOD_d, norm1_d, win_d, cst_d, PF_d, PZ_d, PG_d):
    NB, TB = cfg.NB, cfg.TB
    with ExitStack() as st:
        C = Ctx(nc, st)
        cst = load_consts(P, C, cst_d)
        w = C.sb([128, 8, WIN_COLS], name="win")
        for k in range(8):
            P.dma(lambda e, k=k: e.dma_start(out=r32(w[:, k, :]), in_=r32(win_d[k * 128:(k + 1) * 128, :])), r=[win_d], w=[w])
        xts = [C.sb([128, 1024], name="xt") for _ in range(2)]
        h = C.sb([128, 1024], name="h")
        hT = C.sb([128, 8, 512], name="hT")
        stg = [C.sb([128, 512], name="stg") for _ in range(5)]
        stz = [C.sb([128, 576], name="stz") for _ in range(2)]
        pss = [C.ps(name="pps") for _ in range(6)]
        pst = [C.ps(name="ptr") for _ in range(2)]
        ev = [0]
        pi = [0]
        for b in range(NB):
            with ExitStack() as st2:
                C2 = Ctx(nc, st2)
                rows = {}
                for isc in (False, True):
                    bb = NB if isc else b
                    A = load_mod_rows(P, C2, MOD_d, 0, bb, 1, norm1_d, plus1=True)
                    SH = load_mod_rows(P, C2, MOD_d, 0, bb, 0)
                    rows[isc] = (A, SH)
                ti = 0
                for (t0, n, isc) in token_blocks(cfg):
                    A, SH = rows[isc]
                    for s in range(n // 128):
                        xt = xts[ti % 2]
                        ti += 1
                        r0 = b * TB + t0 + s * 128
                        P.dma(lambda e, xt=xt, r0=r0: e.dma_start(out=xt[:, :], in_=XR_d[r0:r0 + 128, :]),
                              r=[XR_d], w=[xt])
                        norm_mod_tile(P, C2, xt, 128, A, SH, h)
                        transpose_tile(P, h, (0, 1024), 128, hT,
                                       lambda g, nn, s=s: hT[:, g * 4:g * 4 + nn, s * 128:(s + 1) * 128], cst, pst, ev, rnd=True)
                    for fc in range(24):
                        ps = pss[pi[0] % 6]
                        sg = stg[pi[0] % 5]
                        pi[0] += 1
                        for k in range(8):
                            P.op("pe", lambda e, ps=ps, k=k, fc=fc, n=n: e.matmul(
                                ps[:, :n], lhsT=r32(w[:, k, fc * 128:(fc + 1) * 128]), rhs=r32(hT[:, k, :n]),
                                start=(k == 0), stop=(k == 7)), r=[w, hT], w=[ps])
                        if fc % 2 == 0:
                            P.op("act", lambda e, ps=ps, sg=sg, n=n: e.copy(out=sg[:, :n], in_=ps[:, :n]), r=[ps], w=[sg])
                        else:
                            P.op("dve", lambda e, ps=ps, sg=sg, n=n: e.tensor_copy(out=sg[:, :n], in_=ps[:, :n]), r=[ps], w=[sg])
                        P.dma(lambda e, sg=sg, fc=fc, b=b, t0=t0, n=n: e.dma_start(
                            out=PF_d[b, fc * 128:(fc + 1) * 128, t0:t0 + n], in_=sg[:, :n]), r=[sg], w=[PF_d])
                    ps = pss[pi[0] % 6]
                    sg = stg[pi[0] % 5]
                    pi[0] += 1
                    for k in range(8):
                        P.op("pe", lambda e, ps=ps, k=k, n=n: e.matmul(
                            ps[:64, :n], lhsT=r32(w[:, k, 3072 + 512:3072 + 576]), rhs=r32(hT[:, k, :n]),
                            start=(k == 0), stop=(k == 7)), r=[w, hT], w=[ps])
                    P.op("act", lambda e, ps=ps, sg=sg, n=n: e.copy(out=sg[:64, :n], in_=ps[:64, :n]), r=[ps], w=[sg])
                    P.dma(lambda e, sg=sg, b=b, t0=t0, n=n: e.dma_start(out=PG_d[b, :, t0:t0 + n], in_=sg[:64, :n]),
                          r=[sg], w=[PG_d])
                    for s in range(n // 128):
                        ps = pss[pi[0] % 6]
                        ps2 = pss[(pi[0] + 1) % 6]
                        sz = stz[(pi[0] // 2) % 2]
                        pi[0] += 2
                        for k in range(8):
                            P.op("pe", lambda e, ps=ps, k=k, s=s: e.matmul(
                                ps[:, :512], lhsT=r32(hT[:, k, s * 128:(s + 1) * 128]), rhs=r32(w[:, k, 3072:3072 + 512]),
                                start=(k == 0), stop=(k == 7)), r=[w, hT], w=[ps])
                        for k in range(8):
                            P.op("pe", lambda e, ps2=ps2, k=k, s=s: e.matmul(
                                ps2[:, :64], lhsT=r32(hT[:, k, s * 128:(s + 1) * 128]), rhs=r32(w[:, k, 3072 + 512:3072 + 576]),
                                start=(k == 0), stop=(k == 7)), r=[w, hT], w=[ps2])
                        P.op("act", lambda e, ps=ps, sz=sz: e.copy(out=sz[:, :512], in_=ps[:, :512]), r=[ps], w=[sz])
                        P.op("dve", lambda e, ps2=ps2, sz=sz: e.tensor_copy(out=sz[:, 512:576], in_=ps2[:, :64]), r=[ps2], w=[sz])
                        r0 = b * TB + t0 + s * 128
                        P.dma(lambda e, sz=sz, r0=r0: e.dma_start(out=PZ_d[r0:r0 + 128, :], in_=sz[:, :]), r=[sz], w=[PZ_d])
                P.flush("proj0")


def conv3(P, src, dst, wt, ci, ranges):
    w0, w1, w2 = (wt[:, ci, k:k + 1] for k in range(3))
    for (a, b) in ranges:
        P.op("dve", lambda e, a=a, b=b: e.tensor_scalar(out=dst[:, a:b], in0=src[:, a:b], scalar1=w1, scalar2=None,
                                                       op0=ALU.mult), r=[src, wt], w=[dst])
        P.op("dve", lambda e, a=a, b=b: e.scalar_tensor_tensor(out=dst[:, a + 1:b], in0=src[:, a:b - 1], scalar=w0,
                                                               in1=dst[:, a + 1:b], op0=ALU.mult, op1=ALU.add),
             r=[src, wt, dst], w=[dst])
        P.op("dve", lambda e, a=a, b=b: e.scalar_tensor_tensor(out=dst[:, a:b - 1], in0=src[:, a + 1:b], scalar=w2,
                                                               in1=dst[:, a:b - 1], op0=ALU.mult, op1=ALU.add),
             r=[src, wt, dst], w=[dst])


def phase_prep0(nc, P, cfg, cst_d, PF_d, PG_d, convA_d, convQ_d, gpar_d, rm_d,
                YT_d, QT_d, KT_d, KTOK_d, VTOK_d, CB_d):
    NB, TB, L = cfg.NB, cfg.TB, cfg.L
    ranges = [(0, L), (L, TB)]
    nch = TB // 64
    with ExitStack() as st:
        C = Ctx(nc, st)
        cst = load_consts(P, C, cst_d)
        wa = C.sb([128, 4, 3], name="wa")
        wq = C.sb([128, 12, 3], name="wq")
        P.dma(lambda e: e.dma_start(out=wa[:, :, :], in_=convA_d.ap.rearrange("(c p) k -> p c k", p=128)), r=[convA_d], w=[wa])
        P.dma(lambda e: e.dma_start(out=wq[:, :, :], in_=convQ_d.ap.rearrange("(c p) k -> p c k", p=128)), r=[convQ_d], w=[wq])
        gpar = C.sb([16, 2], name="gpar")
        P.dma(lambda e: e.dma_start(out=gpar[:, :], in_=gpar_d[:, :]), r=[gpar_d], w=[gpar])
        rm = C.sb([16, TB], name="rm")
        P.dma(lambda e: e.dma_start(out=rm[:, :], in_=rm_d[:, :]), r=[rm_d], w=[rm])
        negA = C.sb([16, 1], name="negA")
        P.op("act", lambda e: e.activation(out=negA[:, :], in_=gpar[:, 1:2], func=AF.Exp), r=[gpar], w=[negA])
        P.op("dve", lambda e: e.tensor_scalar(out=negA[:, :], in0=negA[:, :], scalar1=-1.0, scalar2=None, op0=ALU.mult),
             r=[negA], w=[negA])
        bufA = [C.sb([128, TB], name="bA") for _ in range(2)]
        bufB = [C.sb([128, TB], name="bB") for _ in range(2)]
        bufC = [C.sb([128, TB], name="bC") for _ in range(2)]
        bufD = [C.sb([128, TB], name="bD") for _ in range(2)]
        sq = C.sb([128, 512], name="sq")
        rn = C.sb([128, 512], name="rn")
        tok = [C.sb([128, 512], name="tok") for _ in range(2)]
        pss = [C.ps(name="pp") for _ in range(2)]
        pst = [C.ps(name="pt") for _ in range(2)]
        ev = [0]
        it = 0
        for b in range(NB):
            ga = C.sb([16, TB], name="ga") if b == 0 else ga
            gb_ = C.sb([16, TB], name="gb") if b == 0 else gb_
            pre = C.sb([16, TB], name="pre") if b == 0 else pre
            suf = C.sb([16, TB], name="suf") if b == 0 else suf
            P.dma(lambda e, b=b: e.dma_start(out=ga[:, :], in_=PG_d[b, 0:16, :]), r=[PG_d], w=[ga])
            P.dma(lambda e, b=b: e.dma_start(out=gb_[:, :], in_=PG_d[b, 32:48, :]), r=[PG_d], w=[gb_])
            P.op("act", lambda e: e.activation(out=ga[:, :], in_=ga[:, :], func=AF.Exp, bias=gpar[:, 0:1]), r=[ga, gpar], w=[ga])
            P.op("act", lambda e: e.activation(out=ga[:, :], in_=ga[:, :], func=AF.Ln, bias=1.0), r=[ga], w=[ga])
            P.op("dve", lambda e: e.tensor_scalar(out=ga[:, :], in0=ga[:, :], scalar1=negA[:, 0:1], scalar2=None, op0=ALU.mult),
                 r=[ga, negA], w=[ga])
            P.op("dve", lambda e: e.tensor_tensor_scan(out=pre[:, :], data0=rm[:, :], data1=ga[:, :], initial=0.0,
                                                       op0=ALU.mult, op1=ALU.add), r=[rm, ga], w=[pre])
            P.op("dve", lambda e: e.tensor_tensor(out=suf[:, :], in0=ga[:, :], in1=pre[:, :], op=ALU.subtract), r=[ga, pre], w=[suf])
            P.op("dve", lambda e: e.tensor_tensor(
                out=suf[:, :].rearrange("p (c t) -> p c t", t=64), in0=suf[:, :].rearrange("p (c t) -> p c t", t=64),
                in1=pre[:, :].rearrange("p (c t) -> p c t", t=64)[:, :, 63:64].to_broadcast([16, nch, 64]), op=ALU.add),
                r=[suf, pre], w=[suf])
            P.op("act", lambda e: e.activation(out=gb_[:, :], in_=gb_[:, :], func=AF.Sigmoid), r=[gb_], w=[gb_])
            P.dma(lambda e, b=b: e.dma_start(out=CB_d[b, 0:8, :], in_=pre[0:8, :]), r=[pre], w=[CB_d])
            P.dma(lambda e, b=b: e.dma_start(out=CB_d[b, 8:16, :], in_=suf[8:16, :]), r=[suf], w=[CB_d])
            P.dma(lambda e, b=b: e.dma_start(out=CB_d[b, 16:32, :], in_=gb_[0:16, :]), r=[gb_], w=[CB_d])
            for j in range(4):
                xa, gcv, gbt, y = bufA[it % 2], bufB[it % 2], bufC[it % 2], bufD[it % 2]
                it += 1
                for t, ch in ((xa, j), (gcv, 8 + j), (gbt, 4 + j)):
                    P.dma(lambda e, t=t, ch=ch, b=b: e.dma_start(out=t[:, :], in_=PF_d[b, ch * 128:(ch + 1) * 128, :]),
                          r=[PF_d], w=[t])
                P.op("dve", lambda e, xa=xa, gcv=gcv: e.tensor_tensor(out=xa[:, :], in0=xa[:, :], in1=gcv[:, :], op=ALU.mult),
                     r=[xa, gcv], w=[xa])
                conv3(P, xa, y, wa, j, ranges)
                P.op("dve", lambda e, y=y, gbt=gbt: e.tensor_tensor(out=y[:, :], in0=y[:, :], in1=gbt[:, :], op=ALU.mult),
                     r=[y, gbt], w=[y])
                P.dma(lambda e, y=y, j=j, b=b: e.dma_start(out=YT_d[b, j * 128:(j + 1) * 128, :], in_=y[:, :]), r=[y], w=[YT_d])
            for cq in range(12):
                x, y = bufA[it % 2], bufD[it % 2]
                it += 1
                P.dma(lambda e, x=x, cq=cq, b=b: e.dma_start(out=x[:, :], in_=PF_d[b, (12 + cq) * 128:(13 + cq) * 128, :]),
                      r=[PF_d], w=[x])
                conv3(P, x, y, wq, cq, ranges)
                P.op("act", lambda e, y=y: e.activation(out=y[:, :], in_=y[:, :], func=AF.Silu), r=[y], w=[y])
                kind = cq // 4
                if kind < 2:
                    for c0 in range(0, TB, 512):
                        n = min(512, TB - c0)
                        ps = pss[(c0 // 512) % 2]
                        P.op("act", lambda e, y=y, c0=c0, n=n: e.activation(out=sq[:, :n], in_=y[:, c0:c0 + n], func=AF.Square),
                             r=[y], w=[sq])
                        P.op("pe", lambda e, ps=ps, n=n: e.matmul(ps[:, :n], lhsT=cst[:, C_BO:C_BO + 128], rhs=sq[:, :n],
                                                                  start=True, stop=True), r=[cst, sq], w=[ps])
                        P.op("act", lambda e, ps=ps, n=n: e.activation(out=rn[:, :n], in_=ps[:, :n], func=AF.Sqrt, bias=1e-6),
                             r=[ps], w=[rn])
                        P.op("dve", lambda e, n=n: e.reciprocal(out=rn[:, :n], in_=rn[:, :n]), r=[rn], w=[rn])
                        sc = 0.125 if kind == 0 else 1.0
                        P.op("dve", lambda e, y=y, c0=c0, n=n, sc=sc: e.scalar_tensor_tensor(
                            out=y[:, c0:c0 + n], in0=y[:, c0:c0 + n], scalar=sc, in1=rn[:, :n], op0=ALU.mult, op1=ALU.mult),
                            r=[y, rn], w=[y])
                    dst = QT_d if kind == 0 else KT_d
                    cc = cq % 4
                    P.dma(lambda e, y=y, dst=dst, cc=cc, b=b: e.dma_start(out=dst[b, cc * 128:(cc + 1) * 128, :], in_=y[:, :]),
                          r=[y], w=[dst])
                if kind >= 1:
                    dstT = KTOK_d if kind == 1 else VTOK_d
                    cc = cq % 4
                    for t0 in range(0, TB, 512):
                        nt = min(4, (TB - t0) // 128)
                        ps = pst[ev[0] % 2]
                        tk = tok[ev[0] % 2]
                        ev[0] += 1
                        for q in range(nt):
                            P.op("pe", lambda e, ps=ps, y=y, q=q, t0=t0: e.transpose(
                                out=ps[:, q * 128:(q + 1) * 128], in_=y[:, t0 + q * 128:t0 + (q + 1) * 128],
                                identity=cst[:, C_ID:C_ID + 128]), r=[y, cst], w=[ps])
                        P.op("act", lambda e, ps=ps, tk=tk, nt=nt: e.copy(out=tk[:, :nt * 128], in_=ps[:, :nt * 128]), r=[ps], w=[tk])
                        P.dma(lambda e, tk=tk, dstT=dstT, b=b, t0=t0, nt=nt, cc=cc: e.dma_start(
                            out=dstT[b * TB + t0:b * TB + t0 + nt * 128, cc * 128:(cc + 1) * 128].rearrange("(q p) f -> p q f", p=128),
                            in_=tk[:, :nt * 128].rearrange("p (q f) -> p q f", f=128)), r=[tk], w=[dstT])
        P.flush("prep0")


def bc_h(ap2, n):
    return ap2.rearrange("p (h o) -> p h o", o=1).to_broadcast([ap2.shape[0], 8, n])


def bc_m(ap2):
    return ap2.rearrange("p (o j) -> p o j", o=1).to_broadcast([ap2.shape[0], 8, 64])


def v3(ap, n=64):
    return ap.rearrange("p (h j) -> p h j", j=n)


def phase_gdn(nc, P, cfg, cst_d, QT_d, KT_d, KTOK_d, VTOK_d, CB_d, O_ds):
    NB, TB, L, NCTX = cfg.NB, cfg.TB, cfg.L, cfg.NCTX
    with ExitStack() as st:
        C = Ctx(nc, st)
        cst = load_consts(P, C, cst_d)
        I64 = cst[:64, C_ID:C_ID + 64]
        NBUF = 2

        class F:
            def __init__(self, fn, r):
                self.fn, self.r = fn, r

            def __call__(self, h):
                return self.fn(h)

        def mm8(ps, lf, rf, n=64):
            for h in range(8):
                P.op("pe", lambda e, h=h: e.matmul(ps[:64, h * n:(h + 1) * n], lhsT=lf(h), rhs=rf(h), start=True, stop=True),
                     r=lf.r + rf.r, w=[ps])

        def tr8(ps, src):
            for h in range(8):
                P.op("pe", lambda e, h=h: e.transpose(out=ps[:64, h * 64:(h + 1) * 64], in_=src(h), identity=I64),
                     r=src.r + [cst], w=[ps])

        def make_tiles():
            t = {}
            t["bufs"] = [(C.sb([64, 8, 64], name="qT"), C.sb([64, 8, 64], name="kT"), C.sb([32, 64], name="cb"),
                          C.sb([64, 8, 64], name="ktok"), C.sb([64, 8, 64], name="vtok"),
                          C.sb([64, 8, 64], name="crow"), C.sb([64, 8, 64], name="brow")) for _ in range(NBUF)]
            t["Zk"] = [C.sb([64, 8, 64], name="Zk") for _ in range(2)]
            for nm in ("S", "D3", "E3", "BM", "D3T", "E3T", "BMT", "ub", "attnT", "wT", "vnew", "kg", "tmpo"):
                t[nm] = C.sb([64, 8, 64], name=nm)
            t["cbt"] = C.sb([64, 32], name="cbt")
            t["Pk"] = [C.sb([64, 8, 64], name="Pk") for _ in range(5)]
            t["Qk"] = [C.sb([64, 8, 64], name="Qk") for _ in range(6)]
            t["Y"] = [C.sb([64, 8, 128], name="Y") for _ in range(2)]
            t["ot"] = [C.sb([64, 8, 64], name="ot") for _ in range(2)]
            t["sm"] = C.sb([64, 64], name="sm")
            t["psY"] = C.ps([128, 1024], name="psY")
            t["pss"] = [C.ps(name="pg") for _ in range(2)]
            return t

        tiles = [make_tiles() for _ in range(2)]

        def stream(b, d, t):
            bufs, S, D3, E3, BM, attnT, wT, vnew, kg, tmpo = (t[k] for k in ("bufs", "S", "D3", "E3", "BM", "attnT", "wT", "vnew", "kg", "tmpo"))
            cbt, Pk, Qk, Y, ot, sm, psY, pss = (t[k] for k in ("cbt", "Pk", "Qk", "Y", "ot", "sm", "psY", "pss"))
            pi = [0]

            def nps():
                p = pss[pi[0] % len(pss)]
                pi[0] += 1
                return p

            MINC = cst[:64, (C_MINC0 if d == 0 else C_MINC1):(C_MINC0 if d == 0 else C_MINC1) + 64]
            MST = cst[:64, (C_MST0 if d == 0 else C_MST1):(C_MST0 if d == 0 else C_MST1) + 64]
            lc = 63 if d == 0 else 0
            cch = [L + i * 64 for i in range(NCTX // 64)]
            lch = [i * 64 for i in range(L // 64)]
            seq = (cch + lch) if d == 0 else (cch[::-1] + lch[::-1])
            O_d = O_ds[d]
            ccol = cbt[:, d * 8:(d + 1) * 8]
            bcol = cbt[:, 16 + d * 8:16 + (d + 1) * 8]

            def loads(ci):
                q_, k_, cb_, kt_, vt_, cr_, br_ = bufs[ci % NBUF]
                t0 = seq[ci]
                r0 = b * TB + t0
                P.dma(lambda e: e.dma_start(out=q_[:, :, :], in_=QT_d[b, :, t0:t0 + 64].rearrange("(h p) t -> p h t", p=64)), r=[QT_d], w=[q_])
                P.dma(lambda e: e.dma_start(out=k_[:, :, :], in_=KT_d[b, :, t0:t0 + 64].rearrange("(h p) t -> p h t", p=64)), r=[KT_d], w=[k_])
                P.dma(lambda e: e.dma_start(out=cb_[:, :], in_=CB_d[b, :, t0:t0 + 64]), r=[CB_d], w=[cb_])
                P.dma(lambda e: e.dma_start(out=kt_[:, :, :], in_=KTOK_d[r0:r0 + 64, :].rearrange("p (h f) -> p h f", f=64)), r=[KTOK_d], w=[kt_])
                P.dma(lambda e: e.dma_start(out=vt_[:, :, :], in_=VTOK_d[r0:r0 + 64, :].rearrange("p (h f) -> p h f", f=64)), r=[VTOK_d], w=[vt_])
                P.dma(lambda e: e.dma_start(out=cr_[:, :, :], in_=CB_d[b, d * 8:(d + 1) * 8, t0:t0 + 64].partition_broadcast(64)), r=[CB_d], w=[cr_])
                P.dma(lambda e: e.dma_start(out=br_[:, :, :], in_=CB_d[b, 16 + d * 8:16 + (d + 1) * 8, t0:t0 + 64].partition_broadcast(64)), r=[CB_d], w=[br_])

            MINCT = cst[:64, (C_MINC1 if d == 0 else C_MINC0):(C_MINC1 if d == 0 else C_MINC0) + 64]
            MSTT = cst[:64, (C_MST1 if d == 0 else C_MST0):(C_MST1 if d == 0 else C_MST0) + 64]
            D3T, E3T, BMT, Zk, ub = t["D3T"], t["E3T"], t["BMT"], t["Zk"], t["ub"]
            P.op("dve", lambda e: e.memset(S[:, :, :], 0.0), r=[], w=[S])
            loads(0)
            for ci, t0 in enumerate(seq):
                q_, k_, cb_, kt_, vt_, cr_, br_ = bufs[ci % NBUF]
                r0 = b * TB + t0
                if ci + 1 < len(seq):
                    loads(ci + 1)
                yield
                p0 = nps()
                P.op("pe", lambda e, p0=p0, cb_=cb_: e.transpose(out=p0[:64, :32], in_=cb_[:32, :], identity=cst[:32, C_ID:C_ID + 32]),
                     r=[cb_, cst], w=[p0])
                P.op("act", lambda e, p0=p0: e.copy(out=cbt[:, :], in_=p0[:64, :32]), r=[p0], w=[cbt])
                P.op("dve", lambda e, cr_=cr_: e.tensor_tensor(out=D3[:, :, :], in0=bc_h(ccol, 64), in1=cr_[:, :, :], op=ALU.subtract), r=[cbt, cr_], w=[D3])
                P.op("dve", lambda e, cr_=cr_: e.tensor_tensor(out=D3T[:, :, :], in0=cr_[:, :, :], in1=bc_h(ccol, 64), op=ALU.subtract), r=[cbt, cr_], w=[D3T])
                P.op("act", lambda e, cr_=cr_: e.copy(out=sm[:, 16:24], in_=cr_[:, :, lc]), r=[cr_], w=[sm])
                P.op("dve", lambda e: e.tensor_tensor(out=D3[:, :, :], in0=D3[:, :, :], in1=bc_m(MINC), op=ALU.add), r=[D3, cst], w=[D3])
                P.op("dve", lambda e: e.tensor_tensor(out=D3T[:, :, :], in0=D3T[:, :, :], in1=bc_m(MINCT), op=ALU.add), r=[D3T, cst], w=[D3T])
                P.op("act", lambda e: e.activation(out=E3[:, :, :], in_=D3[:, :, :], func=AF.Exp), r=[D3], w=[E3])
                P.op("act", lambda e: e.activation(out=E3T[:, :, :], in_=D3T[:, :, :], func=AF.Exp), r=[D3T], w=[E3T])
                P.op("dve", lambda e: e.tensor_tensor(out=BM[:, :, :], in0=bc_m(MST), in1=bc_h(bcol, 64), op=ALU.mult), r=[cst, cbt], w=[BM])
                P.op("dve", lambda e, br_=br_: e.tensor_tensor(out=BMT[:, :, :], in0=br_[:, :, :], in1=bc_m(MSTT), op=ALU.mult), r=[cst, br_], w=[BMT])
                yield
                pk = nps()
                mm8(pk, F(lambda h, k_=k_: k_[:, h, :], [k_]), F(lambda h, k_=k_: k_[:, h, :], [k_]))
                pq = nps()
                mm8(pq, F(lambda h, k_=k_: k_[:, h, :], [k_]), F(lambda h, q_=q_: q_[:, h, :], [q_]))
                P0, Q0 = Pk[0], Qk[0]
                P.op("dve", lambda e, pk=pk: e.tensor_tensor(out=P0[:, :, :], in0=v3(pk[:64, :]), in1=E3[:, :, :], op=ALU.mult), r=[pk, E3], w=[P0])
                P.op("dve", lambda e, pk=pk: e.tensor_tensor(out=Q0[:, :, :], in0=v3(pk[:64, :]), in1=E3T[:, :, :], op=ALU.mult), r=[pk, E3T], w=[Q0])
                P.op("dve", lambda e, pq=pq: e.tensor_tensor(out=attnT[:, :, :], in0=v3(pq[:64, :]), in1=E3T[:, :, :], op=ALU.mult), r=[pq, E3T], w=[attnT])
                P.op("dve", lambda e: e.tensor_tensor(out=P0[:, :, :], in0=P0[:, :, :], in1=BM[:, :, :], op=ALU.mult), r=[P0, BM], w=[P0])
                P.op("dve", lambda e: e.tensor_tensor(out=Q0[:, :, :], in0=Q0[:, :, :], in1=BMT[:, :, :], op=ALU.mult), r=[Q0, BMT], w=[Q0])
                yield
                Y0 = Y[0]
                P.op("act", lambda e: e.activation(out=sm[:, 0:8], in_=ccol, func=AF.Exp), r=[cbt], w=[sm])
                P.op("dve", lambda e: e.tensor_tensor(out=sm[:, 8:16], in0=sm[:, 0:8], in1=bcol, op=ALU.mult), r=[sm, cbt], w=[sm])
                P.op("dve", lambda e, vt_=vt_: e.tensor_tensor(out=Y0[:, :, 0:64], in0=vt_[:, :, :], in1=bc_h(bcol, 64), op=ALU.mult), r=[vt_, cbt], w=[Y0])
                P.op("dve", lambda e, kt_=kt_: e.tensor_tensor(out=Y0[:, :, 64:128], in0=kt_[:, :, :], in1=bc_h(sm[:, 8:16], 64), op=ALU.mult), r=[kt_, sm], w=[Y0])
                Z = Zk[0]
                P.op("dve", lambda e, Z=Z: e.tensor_tensor(out=Z[:, :, :], in0=bc_m(I64), in1=Q0[:, :, :], op=ALU.subtract), r=[cst, Q0], w=[Z])
                yield
                zi = 0
                for lvl in range(5):
                    Pc, Qc = Pk[lvl], Qk[lvl]
                    pb = nps()
                    mm8(pb, F(lambda h, Pc=Pc: Pc[:, h, :], [Pc]), F(lambda h, Qc=Qc: Qc[:, h, :], [Qc]))
                    Qn = Qk[lvl + 1]
                    P.op("dve", lambda e, pb=pb, Qn=Qn: e.tensor_copy(out=Qn[:, :, :], in_=v3(pb[:64, :])), r=[pb], w=[Qn])
                    if lvl < 4:
                        pa = nps()
                        mm8(pa, F(lambda h, Qc=Qc: Qc[:, h, :], [Qc]), F(lambda h, Pc=Pc: Pc[:, h, :], [Pc]))
                        Pn = Pk[lvl + 1]
                        P.op("act", lambda e, pa=pa, Pn=Pn: e.copy(out=Pn[:, :, :], in_=v3(pa[:64, :])), r=[pa], w=[Pn])
                    if lvl >= 1:
                        Zc, Zn = Zk[zi], Zk[1 - zi]
                        pz = nps()
                        mm8(pz, F(lambda h, Pc=Pc: Pc[:, h, :], [Pc]), F(lambda h, Zc=Zc: Zc[:, h, :], [Zc]))
                        P.op("dve", lambda e, pz=pz, Zc=Zc, Zn=Zn: e.tensor_tensor(out=Zn[:, :, :], in0=Zc[:, :, :], in1=v3(pz[:64, :]), op=ALU.add),
                             r=[Zc, pz], w=[Zn])
                        zi = 1 - zi
                    yield
                Z4 = Zk[zi]
                Q5 = Qk[5]
                Y1 = Y[1]
                for h in range(8):
                    P.op("pe", lambda e, h=h: e.matmul(psY[:64, h * 128:(h + 1) * 128], lhsT=Q5[:, h, :], rhs=Y0[:, h, :], start=True, stop=True),
                         r=[Q5, Y0], w=[psY])
                P.op("dve", lambda e: e.tensor_tensor(out=Y1[:, :, :], in0=Y0[:, :, :], in1=v3(psY[:64, :], 128), op=ALU.add), r=[Y0, psY], w=[Y1])
                yield
                pu = nps()
                mm8(pu, F(lambda h, Z4=Z4: Z4[:, h, :], [Z4]), F(lambda h: Y1[:, h, 0:64], [Y1]))
                pw = nps()
                mm8(pw, F(lambda h: Y1[:, h, 64:128], [Y1]), F(lambda h, Z4=Z4: Z4[:, h, :], [Z4]))
                P.op("act", lambda e, pu=pu: e.copy(out=ub[:, :, :], in_=v3(pu[:64, :])), r=[pu], w=[ub])
                P.op("act", lambda e, pw=pw: e.copy(out=wT[:, :, :], in_=v3(pw[:64, :])), r=[pw], w=[wT])
                yield
                pws = nps()
                mm8(pws, F(lambda h: wT[:, h, :], [wT]), F(lambda h: S[:, h, :], [S]))
                P.op("dve", lambda e, pws=pws: e.tensor_tensor(out=vnew[:, :, :], in0=ub[:, :, :], in1=v3(pws[:64, :]), op=ALU.subtract),
                     r=[ub, pws], w=[vnew])
                yield
                po1 = nps()
                mm8(po1, F(lambda h, q_=q_: q_[:, h, :], [q_]), F(lambda h: S[:, h, :], [S]))
                po2 = nps()
                mm8(po2, F(lambda h: attnT[:, h, :], [attnT]), F(lambda h: vnew[:, h, :], [vnew]))
                o_ = ot[ci % 2]
                P.op("dve", lambda e, po1=po1: e.tensor_tensor(out=tmpo[:, :, :], in0=v3(po1[:64, :]), in1=bc_h(sm[:, 0:8], 64), op=ALU.mult),
                     r=[po1, sm], w=[tmpo])
                P.op("dve", lambda e, po2=po2, o_=o_: e.tensor_tensor(out=o_[:, :, :], in0=tmpo[:, :, :], in1=v3(po2[:64, :]), op=ALU.add),
                     r=[tmpo, po2], w=[o_])
                P.dma(lambda e, o_=o_, O_d=O_d, r0=r0: e.dma_start(out=O_d[r0:r0 + 64, :].rearrange("p (h f) -> p h f", f=64), in_=o_[:, :, :]),
                      r=[o_], w=[O_d])
                yield
                P.op("act", lambda e: e.activation(out=sm[:, 24:32], in_=sm[:, 16:24], func=AF.Exp), r=[sm], w=[sm])
                P.op("dve", lambda e: e.tensor_tensor(out=sm[:, 32:40], in0=sm[:, 16:24], in1=ccol, op=ALU.subtract), r=[sm, cbt], w=[sm])
                P.op("act", lambda e: e.activation(out=sm[:, 32:40], in_=sm[:, 32:40], func=AF.Exp), r=[sm], w=[sm])
                P.op("dve", lambda e, kt_=kt_: e.tensor_tensor(out=kg[:, :, :], in0=kt_[:, :, :], in1=bc_h(sm[:, 32:40], 64), op=ALU.mult),
                     r=[kt_, sm], w=[kg])
                pS = nps()
                mm8(pS, F(lambda h: kg[:, h, :], [kg]), F(lambda h: vnew[:, h, :], [vnew]))
                P.op("dve", lambda e: e.tensor_tensor(out=S[:, :, :], in0=S[:, :, :], in1=bc_h(sm[:, 24:32], 64), op=ALU.mult), r=[S, sm], w=[S])
                P.op("dve", lambda e, pS=pS: e.tensor_tensor(out=S[:, :, :], in0=S[:, :, :], in1=v3(pS[:64, :]), op=ALU.add), r=[S, pS], w=[S])
                yield

        for b in range(NB):
            gens = [stream(b, d, tiles[d]) for d in range(2)]
            alive = list(gens)
            while alive:
                for g in list(alive):
                    try:
                        next(g)
                    except StopIteration:
                        alive.remove(g)
            P.flush("gdn%d" % b)


def head_rms(P, C, src, dst, nh, hd, gain_t, eps, sq, extra_scale=1.0):
    ss = C.sb([128, nh], name="hss")
    s3 = lambda t: t[:, :nh * hd].rearrange("p (h f) -> p h f", f=hd)
    P.op("act", lambda e: e.activation(out=sq[:, :nh * hd], in_=src[:, :nh * hd], func=AF.Square), r=[src], w=[sq])
    P.op("dve", lambda e: e.tensor_reduce(out=ss[:, :], in_=s3(sq), axis=AX.X, op=ALU.add), r=[sq], w=[ss])
    P.op("dve", lambda e: e.tensor_scalar(out=ss[:, :], in0=ss[:, :], scalar1=1.0 / hd, scalar2=eps, op0=ALU.mult, op1=ALU.add), r=[ss], w=[ss])
    P.op("act", lambda e: e.activation(out=ss[:, :], in_=ss[:, :], func=AF.Sqrt), r=[ss], w=[ss])
    P.op("dve", lambda e: e.reciprocal(out=ss[:, :], in_=ss[:, :]), r=[ss], w=[ss])
    P.op("dve", lambda e: e.tensor_tensor(out=s3(dst), in0=s3(src), in1=ss[:, :].rearrange("p (h o) -> p h o", o=1).to_broadcast([128, nh, hd]),
                                          op=ALU.mult), r=[src, ss], w=[dst])
    P.op("dve", lambda e: e.scalar_tensor_tensor(out=s3(dst), in0=s3(dst), scalar=extra_scale,
                                                  in1=gain_t[:, :hd].rearrange("p (o f) -> p o f", o=1).to_broadcast([128, nh, hd]),
                                                  op0=ALU.mult, op1=ALU.mult), r=[dst, gain_t], w=[dst])


def phase_merge0(nc, P, cfg, cst_d, OF_d, OB_d, PZ_d, gn_d, YT_d):
    NB, TB = cfg.NB, cfg.TB
    with ExitStack() as st:
        C = Ctx(nc, st)
        cst = load_consts(P, C, cst_d)
        gn = C.sb([128, 64], name="gn")
        P.dma(lambda e: e.dma_start(out=gn[:, :], in_=gn_d.ap.partition_broadcast(128)), r=[gn_d], w=[gn])
        of = [C.sb([128, 512], name="of") for _ in range(3)]
        ob = [C.sb([128, 512], name="ob") for _ in range(3)]
        z = [C.sb([128, 512], name="z") for _ in range(3)]
        sq = C.sb([128, 512], name="sq")
        yT = [C.sb([128, 4, 128], name="yT") for _ in range(3)]
        pst = [C.ps(name="pt") for _ in range(2)]
        ev = [0]
        for b in range(NB):
            for i in range(TB // 128):
                o1, o2, zz, yt = of[i % 3], ob[i % 3], z[i % 3], yT[i % 3]
                r0 = b * TB + i * 128
                P.dma(lambda e, o1=o1, r0=r0: e.dma_start(out=o1[:, :], in_=OF_d[r0:r0 + 128, :]), r=[OF_d], w=[o1])
                P.dma(lambda e, o2=o2, r0=r0: e.dma_start(out=o2[:, :], in_=OB_d[r0:r0 + 128, :]), r=[OB_d], w=[o2])
                P.dma(lambda e, zz=zz, r0=r0: e.dma_start(out=zz[:, :], in_=PZ_d[r0:r0 + 128, 0:512]), r=[PZ_d], w=[zz])
                P.op("dve", lambda e, o1=o1, o2=o2: e.tensor_tensor(out=o1[:, :], in0=o1[:, :], in1=o2[:, :], op=ALU.add), r=[o1, o2], w=[o1])
                head_rms(P, C, o1, o2, 8, 64, gn, 1e-6, sq)
                P.op("act", lambda e, zz=zz: e.activation(out=zz[:, :], in_=zz[:, :], func=AF.Silu), r=[zz], w=[zz])
                P.op("dve", lambda e, o2=o2, zz=zz: e.tensor_tensor(out=o2[:, :], in0=o2[:, :], in1=zz[:, :], op=ALU.mult), r=[o2, zz], w=[o2])
                transpose_tile(P, o2, (0, 512), 128, yt, lambda g, nn, yt=yt: yt[:, 0:nn, :], cst, pst, ev)
                P.dma(lambda e, yt=yt, b=b, i=i: e.dma_start(
                    out=YT_d[b, 512:1024, i * 128:(i + 1) * 128].rearrange("(c p) t -> p c t", p=128), in_=yt[:, :, :]), r=[yt], w=[YT_d])
        P.flush("merge0")


def phase_outproj(nc, P, cfg, YT_d, w_d, MOD_d, layer, XR_d, with_ctx):
    NB, TB = cfg.NB, cfg.TB
    with ExitStack() as st:
        C = Ctx(nc, st)
        w = C.sb([128, 8, 1024], name="wo")
        P.dma(lambda e: e.dma_start(out=r32(w[:, :, :]), in_=r32(w_d.ap.rearrange("(k p) n -> p k n", p=128))), r=[w_d], w=[w])
        yT = [C.sb([128, 8, 512], name="yT") for _ in range(2)]
        xt = [C.sb([128, 1024], name="xt") for _ in range(4)]
        tmp = C.sb([128, 1024], name="tmp")
        pss = [C.ps(name="po") for _ in range(4)]
        bi = 0
        ti = 0
        for b in range(NB):
            with ExitStack() as st2:
                C2 = Ctx(nc, st2)
                G = {False: load_mod_rows(P, C2, MOD_d, layer, b, 2)}
                if with_ctx:
                    G[True] = load_mod_rows(P, C2, MOD_d, layer, NB, 2)
                for (t0, n, isc) in token_blocks(cfg):
                    if isc and not with_ctx:
                        continue
                    y = yT[bi % 2]
                    bi += 1
                    P.dma(lambda e, y=y, b=b, t0=t0, n=n: e.dma_start(
                        out=r32(y[:, :, :n]), in_=r32(YT_d[b, :, t0:t0 + n].rearrange("(k p) t -> p k t", p=128))), r=[YT_d], w=[y])
                    for s in range(n // 128):
                        x = xt[ti % 4]
                        p0, p1 = pss[(ti % 2) * 2], pss[(ti % 2) * 2 + 1]
                        ti += 1
                        r0 = b * TB + t0 + s * 128
                        P.dma(lambda e, x=x, r0=r0: e.dma_start(out=x[:, :], in_=XR_d[r0:r0 + 128, :]), r=[XR_d], w=[x])
                        for half, ps in ((0, p0), (1, p1)):
                            for k in range(8):
                                P.op("pe", lambda e, ps=ps, y=y, k=k, s=s, half=half: e.matmul(
                                    ps[:, :512], lhsT=r32(y[:, k, s * 128:(s + 1) * 128]), rhs=r32(w[:, k, half * 512:(half + 1) * 512]),
                                    start=(k == 0), stop=(k == 7)), r=[y, w], w=[ps])
                            g = G[isc]
                            P.op("dve", lambda e, ps=ps, g=g, half=half: e.tensor_tensor(
                                out=tmp[:, half * 512:(half + 1) * 512], in0=ps[:, :512], in1=g[:, half * 512:(half + 1) * 512], op=ALU.mult),
                                r=[ps, g], w=[tmp])
                        P.op("pool", lambda e, x=x: e.tensor_tensor(out=x[:, :], in0=x[:, :], in1=tmp[:, :], op=ALU.add), r=[x, tmp], w=[x])
                        P.dma(lambda e, x=x, r0=r0: e.dma_start(out=XR_d[r0:r0 + 128, :], in_=x[:, :]), r=[x], w=[XR_d])
                P.flush("outproj")


def moe_tokens(cfg, with_ctx):
    out = []
    for b in range(cfg.NB):
        n = cfg.TB if with_ctx else cfg.L
        for t0 in range(0, n, 128):
            out.append((b, t0, t0 >= cfg.L))
    return out


def phase_route(nc, P, cfg, cst_d, XR_d, MOD_d, layer, norm2_d, wr_d, XB_d, RTI_d, RTW_d, with_ctx):
    NB, TB, CAP = cfg.NB, cfg.TB, cfg.CAP
    with ExitStack() as st:
        C = Ctx(nc, st)
        cst = load_consts(P, C, cst_d)
        wr = C.sb([128, 8, 72], name="wr")
        P.dma(lambda e: e.dma_start(out=wr[:, :, :], in_=wr_d.ap.rearrange("(k p) n -> p k n", p=128)), r=[wr_d], w=[wr])
        zt = C.sb([128, 4096], name="zt")
        P.op("pool", lambda e: e.memset(zt[:, :], 0.0), r=[], w=[zt])
        tot = 64 * CAP * 1024
        per = 128 * 4096
        XBf = XB_d.ap.rearrange("s d -> (s d)")
        for i in range(tot // per if layer == 0 else 0):
            P.dma(lambda e, i=i: e.dma_start(out=XBf[i * per:(i + 1) * per].rearrange("(p f) -> p f", p=128), in_=zt[:, :]),
                  r=[zt], w=[XB_d])
        cnt = C.sb([128, 64], name="cnt")
        P.op("dve", lambda e: e.memset(cnt[:, :], 0.0), r=[], w=[cnt])
        xts = [C.sb([128, 1024], name="xt") for _ in range(3)]
        hs = [C.sb([128, 1024], name="h2") for _ in range(4)]
        pst = [C.ps(name="pt") for _ in range(2)]
        ev = [0]

        def mk():
            d_ = dict(hT=C.sb([128, 8, 128], name="hT"), lg=C.sb([128, 72], name="lg"), psl=C.ps(name="pl"), psp=C.ps(name="pp"),
                      psc=C.ps(name="pc"), sm=C.sb([128, 16], name="sm"), ohg=C.sb([128, 8], name="ohg"), t88=C.sb([128, 8, 8], name="t88"),
                      ein=C.sb([128, 8], name="ein"), oh1=C.sb([128, 8], name="oh1"), oh2=C.sb([128, 8], name="oh2"),
                      msk=C.sb([128, 8], name="msk"), OH1=C.sb([128, 64], name="OH1"), OH2=C.sb([128, 64], name="OH2"),
                      M=C.sb([128, 64], name="M"), pos=C.sb([128, 64], name="pos"), t64=C.sb([128, 64], name="t64"))
            return d_

        sets = [mk(), mk()]
        def do_tile(b, t0, isc, A, SH, xt, h, S_, C2):
            hT, lg, psl, psp, psc, sm, ohg, t88, ein, oh1, oh2, msk, OH1, OH2, M, pos, t64 = (S_[k] for k in (
                "hT", "lg", "psl", "psp", "psc", "sm", "ohg", "t88", "ein", "oh1", "oh2", "msk", "OH1", "OH2", "M", "pos", "t64"))
            r0 = b * TB + t0
            P.dma(lambda e, xt=xt, r0=r0: e.dma_start(out=xt[:, :], in_=XR_d[r0:r0 + 128, :]), r=[XR_d], w=[xt])
            norm_mod_tile(P, C2, xt, 128, A, SH, h)
            transpose_tile(P, h, (0, 1024), 128, hT, lambda g, nn: hT[:, g * 4:g * 4 + nn, :], cst, pst, ev)
            for k in range(8):
                P.op("pe", lambda e, k=k: e.matmul(psl[:, :72], lhsT=hT[:, k, :], rhs=wr[:, k, :], start=(k == 0), stop=(k == 7)),
                     r=[hT, wr], w=[psl])
            P.op("act", lambda e: e.copy(out=lg[:, :], in_=psl[:, :72]), r=[psl], w=[lg])
            le3 = lg[:, 8:72].rearrange("p (g e) -> p g e", e=8)
            P.op("dve", lambda e: e.tensor_reduce(out=sm[:, 0:1], in_=lg[:, 0:8], axis=AX.X, op=ALU.max), r=[lg], w=[sm])
            P.op("dve", lambda e: e.tensor_scalar(out=ohg[:, :], in0=lg[:, 0:8], scalar1=sm[:, 0:1], scalar2=None, op0=ALU.is_equal), r=[lg, sm], w=[ohg])
            P.op("dve", lambda e: e.tensor_scalar(out=sm[:, 1:2], in0=sm[:, 0:1], scalar1=-1.0, scalar2=None, op0=ALU.mult), r=[sm], w=[sm])
            P.op("act", lambda e: e.activation(out=msk[:, :], in_=lg[:, 0:8], func=AF.Exp, bias=sm[:, 1:2], accum_out=sm[:, 2:3]), r=[lg, sm], w=[msk, sm])
            P.op("dve", lambda e: e.reciprocal(out=sm[:, 3:4], in_=sm[:, 2:3]), r=[sm], w=[sm])
            P.op("dve", lambda e: e.tensor_tensor(out=t88[:, :, :], in0=le3, in1=ohg[:, :].rearrange("p (g o) -> p g o", o=1).to_broadcast([128, 8, 8]),
                                                  op=ALU.mult), r=[lg, ohg], w=[t88])
            P.op("dve", lambda e: e.tensor_reduce(out=ein[:, :], in_=t88[:, :, :].rearrange("p g e -> p e g"), axis=AX.X, op=ALU.add), r=[t88], w=[ein])
            P.op("dve", lambda e: e.tensor_reduce(out=sm[:, 4:5], in_=ein[:, :], axis=AX.X, op=ALU.max), r=[ein], w=[sm])
            P.op("dve", lambda e: e.tensor_scalar(out=oh1[:, :], in0=ein[:, :], scalar1=sm[:, 4:5], scalar2=None, op0=ALU.is_equal), r=[ein, sm], w=[oh1])
            P.op("dve", lambda e: e.scalar_tensor_tensor(out=msk[:, :], in0=oh1[:, :], scalar=-1e30, in1=ein[:, :], op0=ALU.mult, op1=ALU.add), r=[oh1, ein], w=[msk])
            P.op("dve", lambda e: e.tensor_reduce(out=sm[:, 5:6], in_=msk[:, :], axis=AX.X, op=ALU.max), r=[msk], w=[sm])
            P.op("dve", lambda e: e.tensor_scalar(out=oh2[:, :], in0=msk[:, :], scalar1=sm[:, 5:6], scalar2=None, op0=ALU.is_equal), r=[msk, sm], w=[oh2])
            P.op("dve", lambda e: e.tensor_tensor(out=sm[:, 6:7], in0=sm[:, 5:6], in1=sm[:, 4:5], op=ALU.subtract), r=[sm], w=[sm])
            P.op("act", lambda e: e.activation(out=sm[:, 6:7], in_=sm[:, 6:7], func=AF.Exp), r=[sm], w=[sm])
            P.op("dve", lambda e: e.tensor_scalar(out=sm[:, 6:7], in0=sm[:, 6:7], scalar1=1.0, scalar2=None, op0=ALU.add), r=[sm], w=[sm])
            P.op("dve", lambda e: e.reciprocal(out=sm[:, 6:7], in_=sm[:, 6:7]), r=[sm], w=[sm])
            P.op("dve", lambda e: e.tensor_tensor(out=sm[:, 8:9], in0=sm[:, 6:7], in1=sm[:, 3:4], op=ALU.mult), r=[sm], w=[sm])
            P.op("dve", lambda e: e.tensor_tensor(out=sm[:, 9:10], in0=sm[:, 3:4], in1=sm[:, 8:9], op=ALU.subtract), r=[sm], w=[sm])
            for ohk, OHk in ((oh1, OH1), (oh2, OH2)):
                P.op("dve", lambda e, ohk=ohk, OHk=OHk: e.tensor_tensor(
                    out=OHk[:, :].rearrange("p (g e) -> p g e", e=8), in0=ohg[:, :].rearrange("p (g o) -> p g o", o=1).to_broadcast([128, 8, 8]),
                    in1=ohk[:, :].rearrange("p (o e) -> p o e", o=1).to_broadcast([128, 8, 8]), op=ALU.mult), r=[ohg, ohk], w=[OHk])
            P.op("dve", lambda e: e.tensor_tensor(out=M[:, :], in0=OH1[:, :], in1=OH2[:, :], op=ALU.add), r=[OH1, OH2], w=[M])
            P.op("pe", lambda e: e.matmul(psp[:, :64], lhsT=cst[:, C_UT:C_UT + 128], rhs=M[:, :], start=True, stop=True), r=[cst, M], w=[psp])
            P.op("pe", lambda e: e.matmul(psc[:, :64], lhsT=cst[:, C_ONES:C_ONES + 128], rhs=M[:, :], start=True, stop=True), r=[cst, M], w=[psc])
            P.op("dve", lambda e: e.tensor_tensor(out=pos[:, :], in0=psp[:, :64], in1=cnt[:, :], op=ALU.add), r=[psp, cnt], w=[pos])
            P.op("dve", lambda e: e.tensor_tensor(out=cnt[:, :], in0=psc[:, :64], in1=cnt[:, :], op=ALU.add), r=[psc, cnt], w=[cnt])
            P.op("dve", lambda e: e.scalar_tensor_tensor(out=pos[:, :], in0=cst[:, C_IOTA:C_IOTA + 64], scalar=float(CAP), in1=pos[:, :],
                                                          op0=ALU.mult, op1=ALU.add), r=[cst, pos], w=[pos])
            idx = C2.sb([128, 2], I32, name="idx")
            for kk, OHk in ((0, OH1), (1, OH2)):
                P.op("dve", lambda e, OHk=OHk: e.tensor_tensor(out=t64[:, :], in0=OHk[:, :], in1=pos[:, :], op=ALU.mult), r=[OHk, pos], w=[t64])
                P.op("dve", lambda e, kk=kk: e.tensor_reduce(out=sm[:, 10 + kk:11 + kk], in_=t64[:, :], axis=AX.X, op=ALU.add), r=[t64], w=[sm])
            P.op("dve", lambda e, idx=idx: e.tensor_copy(out=idx[:, :], in_=sm[:, 10:12]), r=[sm], w=[idx])
            for kk in range(2):
                P.dma(lambda e, idx=idx, kk=kk, h=h: e.indirect_dma_start(
                    out=XB_d[:, :], out_offset=bass.IndirectOffsetOnAxis(ap=idx[:, kk:kk + 1], axis=0), in_=h[:, :], in_offset=None),
                    r=[h, idx], w=[XB_d], q="pool")
            P.dma(lambda e, idx=idx, r0=r0: e.dma_start(out=RTI_d[r0:r0 + 128, :], in_=idx[:, :]), r=[idx], w=[RTI_d])
            P.dma(lambda e, r0=r0: e.dma_start(out=RTW_d[r0:r0 + 128, :], in_=sm[:, 8:10]), r=[sm], w=[RTW_d])

        cur_b = -1
        rows = None
        st2 = None
        ti = 0
        for (b, t0, isc) in moe_tokens(cfg, with_ctx):
            if b != cur_b:
                if st2 is not None:
                    P.flush("route")
                    st2.close()
                st2 = ExitStack()
                C2 = Ctx(nc, st2)
                rows = {}
                for ic in ((False, True) if with_ctx else (False,)):
                    bb = NB if ic else b
                    rows[ic] = (load_mod_rows(P, C2, MOD_d, layer, bb, 4, norm2_d, plus1=True), load_mod_rows(P, C2, MOD_d, layer, bb, 3))
                cur_b = b
            A, SH = rows[isc]
            do_tile(b, t0, isc, A, SH, xts[ti % 3], hs[ti % 4], sets[ti % 2], C2)
            ti += 1
        P.flush("route")
        if st2 is not None:
            st2.close()


def phase_experts(nc, P, cfg, cst_d, XB_d, wg_d, wu_d, wd_d, YB_d):
    CAP = cfg.CAP
    NS = CAP // 128
    with ExitStack() as st:
        C = Ctx(nc, st)
        cst = load_consts(P, C, cst_d)
        wg = [C.sb([128, 8, 384], name="wg") for _ in range(2)]
        wu = [C.sb([128, 8, 384], name="wu") for _ in range(2)]
        wd = [C.sb([128, 3, 1024], name="wd") for _ in range(2)]
        xt = [C.sb([128, 1024], name="xt") for _ in range(2 * NS)]
        xT = C.sb([128, 8, CAP], name="xT")
        aT = C.sb([128, 3, CAP], name="aT")
        yo = [C.sb([128, 1024], name="yo") for _ in range(4)]
        pst = [C.ps(name="pt") for _ in range(2)]
        pss = [C.ps(name="pe") for _ in range(4)]
        ev = [0]

        def prefetch(ex):
            g_, u_, d_ = wg[ex % 2], wu[ex % 2], wd[ex % 2]
            P.dma(lambda e: e.dma_start(out=r32(g_[:, :, :].rearrange("p k f -> p (k f)")), in_=r32(wg_d[ex])), r=[wg_d], w=[g_])
            P.dma(lambda e: e.dma_start(out=r32(u_[:, :, :].rearrange("p k f -> p (k f)")), in_=r32(wu_d[ex])), r=[wu_d], w=[u_])
            P.dma(lambda e: e.dma_start(out=r32(d_[:, :, :].rearrange("p k f -> p (k f)")), in_=r32(wd_d[ex])), r=[wd_d], w=[d_])
            for s in range(NS):
                x = xt[(ex % 2) * NS + s]
                r0 = ex * CAP + s * 128
                P.dma(lambda e, x=x, r0=r0: e.dma_start(out=x[:, :], in_=XB_d[r0:r0 + 128, :]), r=[XB_d], w=[x])

        prefetch(0)
        for ex in range(cfg.NE):
            g_, u_, d_ = wg[ex % 2], wu[ex % 2], wd[ex % 2]
            if ex + 1 < cfg.NE:
                prefetch(ex + 1)
            for s in range(NS):
                x = xt[(ex % 2) * NS + s]
                transpose_tile(P, x, (0, 1024), 128, xT, lambda g, nn, s=s: xT[:, g * 4:g * 4 + nn, s * 128:(s + 1) * 128], cst, pst, ev, rnd=True)
            for f in range(3):
                pg, pu = pss[(f % 2) * 2], pss[(f % 2) * 2 + 1]
                for k in range(8):
                    P.op("pe", lambda e, pg=pg, g_=g_, k=k, f=f: e.matmul(pg[:, :CAP], lhsT=r32(g_[:, k, f * 128:(f + 1) * 128]), rhs=r32(xT[:, k, :]),
                                                                      start=(k == 0), stop=(k == 7)), r=[g_, xT], w=[pg])
                for k in range(8):
                    P.op("pe", lambda e, pu=pu, u_=u_, k=k, f=f: e.matmul(pu[:, :CAP], lhsT=r32(u_[:, k, f * 128:(f + 1) * 128]), rhs=r32(xT[:, k, :]),
                                                                      start=(k == 0), stop=(k == 7)), r=[u_, xT], w=[pu])
                P.op("act", lambda e, pg=pg, f=f: e.activation(out=r32(aT[:, f, :]), in_=pg[:, :CAP], func=AF.Silu), r=[pg], w=[aT])
                P.op("dve", lambda e, pu=pu, f=f: e.tensor_tensor(out=r32(aT[:, f, :]), in0=aT[:, f, :], in1=pu[:, :CAP], op=ALU.mult), r=[aT, pu], w=[aT])
            for s in range(NS):
                y = yo[s % 4]
                for half in range(2):
                    ps = pss[(s * 2 + half) % 4]
                    for f in range(3):
                        P.op("pe", lambda e, ps=ps, d_=d_, f=f, s=s, half=half: e.matmul(
                            ps[:, :512], lhsT=r32(aT[:, f, s * 128:(s + 1) * 128]), rhs=r32(d_[:, f, half * 512:(half + 1) * 512]),
                            start=(f == 0), stop=(f == 2)), r=[aT, d_], w=[ps])
                    if half == 0:
                        P.op("act", lambda e, ps=ps, y=y: e.copy(out=y[:, 0:512], in_=ps[:, :512]), r=[ps], w=[y])
                    else:
                        P.op("dve", lambda e, ps=ps, y=y: e.tensor_copy(out=y[:, 512:1024], in_=ps[:, :512]), r=[ps], w=[y])
                r0 = ex * CAP + s * 128
                P.dma(lambda e, y=y, r0=r0: e.dma_start(out=YB_d[r0:r0 + 128, :], in_=y[:, :]), r=[y], w=[YB_d])
        P.flush("experts")


def phase_combine(nc, P, cfg, XR_d, MOD_d, layer, YB_d, RTI_d, RTW_d, with_ctx, final=None):
    NB, TB = cfg.NB, cfg.TB
    with ExitStack() as st:
        C = Ctx(nc, st)
        xts = [C.sb([128, 1024], name="xt") for _ in range(3)]
        y1s = [C.sb([128, 1024], name="y1") for _ in range(3)]
        y2s = [C.sb([128, 1024], name="y2") for _ in range(3)]
        fg = None
        if final is not None:
            fg = C.sb([128, 1024], name="fg")
            P.dma(lambda e: e.dma_start(out=fg[:, :], in_=final[0].ap.partition_broadcast(128)), r=[final[0]], w=[fg])
        cur_b = -1
        st2 = None
        G = None
        ti = 0
        for (b, t0, isc) in moe_tokens(cfg, with_ctx):
            if b != cur_b:
                if st2 is not None:
                    P.flush("combine")
                    st2.close()
                st2 = ExitStack()
                C2 = Ctx(nc, st2)
                G = {False: load_mod_rows(P, C2, MOD_d, layer, b, 5)}
                if with_ctx:
                    G[True] = load_mod_rows(P, C2, MOD_d, layer, NB, 5)
                cur_b = b
            g = G[isc]
            x, y1, y2 = xts[ti % 3], y1s[ti % 3], y2s[ti % 3]
            ti += 1
            r0 = b * TB + t0
            idx = C2.sb([128, 2], I32, name="idx")
            wts = C2.sb([128, 2], name="wts")
            P.dma(lambda e, idx=idx, r0=r0: e.dma_start(out=idx[:, :], in_=RTI_d[r0:r0 + 128, :]), r=[RTI_d], w=[idx])
            P.dma(lambda e, wts=wts, r0=r0: e.dma_start(out=wts[:, :], in_=RTW_d[r0:r0 + 128, :]), r=[RTW_d], w=[wts])
            P.dma(lambda e, x=x, r0=r0: e.dma_start(out=x[:, :], in_=XR_d[r0:r0 + 128, :]), r=[XR_d], w=[x])
            for kk, y in ((0, y1), (1, y2)):
                P.dma(lambda e, idx=idx, kk=kk, y=y: e.indirect_dma_start(
                    out=y[:, :], out_offset=None, in_=YB_d[:, :], in_offset=bass.IndirectOffsetOnAxis(ap=idx[:, kk:kk + 1], axis=0)),
                    r=[YB_d, idx], w=[y], q="pool")
            P.op("dve", lambda e, y1=y1, wts=wts: e.tensor_scalar(out=y1[:, :], in0=y1[:, :], scalar1=wts[:, 0:1], scalar2=None, op0=ALU.mult), r=[y1, wts], w=[y1])
            P.op("dve", lambda e, y1=y1, y2=y2, wts=wts: e.scalar_tensor_tensor(out=y1[:, :], in0=y2[:, :], scalar=wts[:, 1:2], in1=y1[:, :],
                                                                              op0=ALU.mult, op1=ALU.add), r=[y1, y2, wts], w=[y1])
            P.op("pool", lambda e, y1=y1, g=g: e.tensor_tensor(out=y1[:, :], in0=y1[:, :], in1=g[:, :], op=ALU.mult), r=[y1, g], w=[y1])
            P.op("dve", lambda e, x=x, y1=y1: e.tensor_tensor(out=x[:, :], in0=x[:, :], in1=y1[:, :], op=ALU.add), r=[x, y1], w=[x])
            if final is None:
                P.dma(lambda e, x=x, r0=r0: e.dma_start(out=XR_d[r0:r0 + 128, :], in_=x[:, :]), r=[x], w=[XR_d])
            else:
                rs = emit_rstd(P, C2, x, 128, 1024, 1e-6, y2)
                P.op("dve", lambda e, x=x, rs=rs: e.scalar_tensor_tensor(out=x[:, :], in0=x[:, :], scalar=rs[:, 0:1], in1=fg[:, :],
                                                                          op0=ALU.mult, op1=ALU.mult), r=[x, rs, fg], w=[x])
                o0 = b * cfg.L + t0
                P.dma(lambda e, x=x, o0=o0: e.dma_start(out=final[1][o0:o0 + 128, :], in_=x[:, :]), r=[x], w=[final[1]])
        P.flush("combine")
        if st2 is not None:
            st2.close()


def rope(P, src, dst, nh, cs, sn, t1, t2):
    s4 = lambda t: t[:, :nh * 128].rearrange("p (h i two) -> p h i two", i=64, two=2)
    cb = cs[:, :].rearrange("p (o i) -> p o i", o=1).to_broadcast([128, nh, 64])
    sb = sn[:, :].rearrange("p (o i) -> p o i", o=1).to_broadcast([128, nh, 64])
    a3 = lambda t: t[:, :nh * 64].rearrange("p (h i) -> p h i", i=64)
    x1, x2 = s4(src)[:, :, :, 0], s4(src)[:, :, :, 1]
    o1, o2 = s4(dst)[:, :, :, 0], s4(dst)[:, :, :, 1]
    P.op("dve", lambda e: e.tensor_tensor(out=a3(t1), in0=x1, in1=cb, op=ALU.mult), r=[src, cs], w=[t1])
    P.op("dve", lambda e: e.tensor_tensor(out=a3(t2), in0=x2, in1=sb, op=ALU.mult), r=[src, sn], w=[t2])
    P.op("dve", lambda e: e.tensor_tensor(out=o1, in0=a3(t1), in1=a3(t2), op=ALU.subtract), r=[t1, t2], w=[dst])
    P.op("dve", lambda e: e.tensor_tensor(out=a3(t1), in0=x1, in1=sb, op=ALU.mult), r=[src, sn], w=[t1])
    P.op("dve", lambda e: e.tensor_tensor(out=a3(t2), in0=x2, in1=cb, op=ALU.mult), r=[src, cs], w=[t2])
    P.op("dve", lambda e: e.tensor_tensor(out=o2, in0=a3(t1), in1=a3(t2), op=ALU.add), r=[t1, t2], w=[dst])


def phase_proj1(nc, P, cfg, cst_d, XR_d, MOD_d, norm1_d, wqkv_d, qn_d, kn_d, cos_d, sin_d, QT1_d, KT1_d, V1_d):
    NB, TB, L = cfg.NB, cfg.TB, cfg.L
    with ExitStack() as st:
        C = Ctx(nc, st)
        cst = load_consts(P, C, cst_d)
        w = C.sb([128, 8, 1536], name="wqkv")
        P.dma(lambda e: e.dma_start(out=r32(w[:, :, :]), in_=r32(wqkv_d.ap.rearrange("(k p) n -> p k n", p=128))), r=[wqkv_d], w=[w])
        qn = C.sb([128, 128], name="qn")
        kn = C.sb([128, 128], name="kn")
        P.dma(lambda e: e.dma_start(out=qn[:, :], in_=qn_d.ap.partition_broadcast(128)), r=[qn_d], w=[qn])
        P.dma(lambda e: e.dma_start(out=kn[:, :], in_=kn_d.ap.partition_broadcast(128)), r=[kn_d], w=[kn])
        pst = [C.ps(name="pt") for _ in range(2)]

        def mk():
            return dict(xt=C.sb([128, 1024], name="xt"), h=C.sb([128, 1024], name="h"), hT=C.sb([128, 8, 128], name="hT"),
                        qkv=C.sb([128, 1536], name="qkv"), qr=C.sb([128, 1024], name="qr"), kr=C.sb([128, 256], name="kr"),
                        kro=C.sb([128, 256], name="kro"), qro=C.sb([128, 1024], name="qro"), sq=C.sb([128, 1024], name="sq"),
                        t1=C.sb([128, 512], name="t1"), t2=C.sb([128, 512], name="t2"), cs=C.sb([128, 64], name="cs"),
                        sn=C.sb([128, 64], name="sn"), qT=C.sb([128, 8, 128], name="qT"), kT=C.sb([128, 2, 128], name="kT"),
                        pss=[C.ps(name="pq") for _ in range(3)])

        sets = [mk(), mk()]
        ev = [0]
        def do_tile(b, t0, isc, A, SH, S_, C2):
            xt, h, hT, qkv, qr, kr, kro, qro, sq, t1, t2, c_, s_, q_T, k_T, pss = (S_[k] for k in (
                "xt", "h", "hT", "qkv", "qr", "kr", "kro", "qro", "sq", "t1", "t2", "cs", "sn", "qT", "kT", "pss"))
            r0 = b * TB + t0
            P.dma(lambda e, xt=xt, r0=r0: e.dma_start(out=xt[:, :], in_=XR_d[r0:r0 + 128, :]), r=[XR_d], w=[xt])
            norm_mod_tile(P, C2, xt, 128, A, SH, h)
            transpose_tile(P, h, (0, 1024), 128, hT, lambda g, nn: hT[:, g * 4:g * 4 + nn, :], cst, pst, ev, rnd=True)
            for c3 in range(3):
                ps = pss[c3]
                for k in range(8):
                    P.op("pe", lambda e, ps=ps, k=k, c3=c3: e.matmul(ps[:, :512], lhsT=r32(hT[:, k, :]), rhs=r32(w[:, k, c3 * 512:(c3 + 1) * 512]),
                                                                  start=(k == 0), stop=(k == 7)), r=[hT, w], w=[ps])
                if c3 % 2 == 0:
                    P.op("act", lambda e, ps=ps, c3=c3: e.copy(out=qkv[:, c3 * 512:(c3 + 1) * 512], in_=ps[:, :512]), r=[ps], w=[qkv])
                else:
                    P.op("dve", lambda e, ps=ps, c3=c3: e.tensor_copy(out=qkv[:, c3 * 512:(c3 + 1) * 512], in_=ps[:, :512]), r=[ps], w=[qkv])
            P.dma(lambda e, r0=r0: e.dma_start(out=V1_d[r0:r0 + 128, :], in_=qkv[:, 1280:1536]), r=[qkv], w=[V1_d])
            kview = T(qkv.ap[:, 1024:1280], "kview")
            kview.key = qkv.key
            head_rms(P, C2, kview, kr, 2, 128, kn, 1e-6, sq)
            ksrc = kr
            if not isc:
                P.dma(lambda e, c_=c_, t0=t0: e.dma_start(out=c_[:, :], in_=cos_d[t0:t0 + 128, :]), r=[cos_d], w=[c_])
                P.dma(lambda e, s_=s_, t0=t0: e.dma_start(out=s_[:, :], in_=sin_d[t0:t0 + 128, :]), r=[sin_d], w=[s_])
                rope(P, kr, kro, 2, c_, s_, t1, t2)
                ksrc = kro
            transpose_tile(P, ksrc, (0, 256), 128, k_T, lambda g, nn, k_T=k_T: k_T[:, 0:nn, :], cst, pst, ev)
            P.dma(lambda e, k_T=k_T, b=b, t0=t0: e.dma_start(out=KT1_d[b, :, t0:t0 + 128].rearrange("(c p) t -> p c t", p=128), in_=k_T[:, :, :]),
                  r=[k_T], w=[KT1_d])
            if not isc:
                head_rms(P, C2, qkv, qr, 8, 128, qn, 1e-6, sq, extra_scale=128 ** -0.5)
                rope(P, qr, qro, 8, c_, s_, t1, t2)
                transpose_tile(P, qro, (0, 1024), 128, q_T, lambda g, nn, q_T=q_T: q_T[:, g * 4:g * 4 + nn, :], cst, pst, ev)
                P.dma(lambda e, q_T=q_T, b=b, t0=t0: e.dma_start(out=QT1_d[b, :, t0:t0 + 128].rearrange("(c p) t -> p c t", p=128), in_=q_T[:, :, :]),
                      r=[q_T], w=[QT1_d])

        ti = 0
        for b in range(NB):
            with ExitStack() as st2:
                C2 = Ctx(nc, st2)
                rows = {}
                for isc in (False, True):
                    bb = NB if isc else b
                    rows[isc] = (load_mod_rows(P, C2, MOD_d, 1, bb, 1, norm1_d, plus1=True), load_mod_rows(P, C2, MOD_d, 1, bb, 0))
                for t0 in range(0, TB, 128):
                    isc = t0 >= L
                    A, SH = rows[isc]
                    do_tile(b, t0, isc, A, SH, sets[ti % 2], C2)
                    ti += 1
                P.flush("proj1")


def phase_attn(nc, P, cfg, cst_d, QT1_d, KT1_d, V1_d, OT_d):
    NB, TB, L = cfg.NB, cfg.TB, cfg.L
    NK = TB // 128
    QG = min(512, L)
    with ExitStack() as st:
        C = Ctx(nc, st)
        cst = load_consts(P, C, cst_d)
        ones_r = C.sb([128, 128], name="ones_r")
        P.dma(lambda e: e.dma_start(out=r32(ones_r[:, :]), in_=r32(cst_d[:, C_ONES:C_ONES + 128])), r=[cst_d], w=[ones_r])
        kT = [C.sb([128, TB], name="kT") for _ in range(2)]
        v = [C.sb([128, NK, 128], name="v") for _ in range(2)]
        qT = [C.sb([128, L], name="qT") for _ in range(2)]
        pT = [C.sb([128, QG], name="pT") for _ in range(3)]
        rd = C.sb([128, QG], name="rd")
        oT = [C.sb([128, QG], name="oT") for _ in range(4)]
        psS = [C.ps(name="psS") for _ in range(2)]
        psO = [C.ps(name="psO") for _ in range(2)]
        psD = [C.ps(name="psD") for _ in range(2)]
        items = [(b, kv, hh) for b in range(NB) for kv in range(2) for hh in range(4)]

        def load_kv(b, kv):
            k_, v_ = kT[(b * 2 + kv) % 2], v[(b * 2 + kv) % 2]
            P.dma(lambda e: e.dma_start(out=r32(k_[:, :]), in_=r32(KT1_d[b, kv * 128:(kv + 1) * 128, :])), r=[KT1_d], w=[k_])
            P.dma(lambda e: e.dma_start(
                out=r32(v_[:, :, :]), in_=r32(V1_d[b * TB:(b + 1) * TB, kv * 128:(kv + 1) * 128].rearrange("(n p) f -> p n f", p=128))), r=[V1_d], w=[v_])

        def load_q(i):
            b, kv, hh = items[i]
            q_ = qT[i % 2]
            hd_ = kv * 4 + hh
            P.dma(lambda e: e.dma_start(out=r32(q_[:, :]), in_=r32(QT1_d[b, hd_ * 128:(hd_ + 1) * 128, :])), r=[QT1_d], w=[q_])

        load_kv(0, 0)
        load_q(0)
        gi = 0
        for i, (b, kv, hh) in enumerate(items):
            k_, v_ = kT[(b * 2 + kv) % 2], v[(b * 2 + kv) % 2]
            q_ = qT[i % 2]
            hd_ = kv * 4 + hh
            if i + 1 < len(items):
                nb_, nkv, nhh = items[i + 1]
                if nhh == 0:
                    load_kv(nb_, nkv)
                load_q(i + 1)
            for q0 in range(0, L, QG):
                pO, pD = psO[gi % 2], psD[gi % 2]
                o_ = oT[gi % 4]
                gi += 1
                def emit_S(kt, k_=k_, q_=q_, q0=q0):
                    pS = psS[kt % 2]
                    P.op("pe", lambda e, pS=pS, k_=k_, q_=q_, kt=kt, q0=q0: e.matmul(
                        pS[:, :QG], lhsT=r32(k_[:, kt * 128:(kt + 1) * 128]), rhs=r32(q_[:, q0:q0 + QG]), start=True, stop=True),
                        r=[k_, q_], w=[pS])

                emit_S(0)
                for kt in range(NK):
                    if kt + 1 < NK:
                        emit_S(kt + 1)
                    pS = psS[kt % 2]
                    p_ = pT[kt % 3]
                    P.op("act", lambda e, pS=pS, p_=p_: e.activation(out=r32(p_[:, :]), in_=pS[:, :QG], func=AF.Exp), r=[pS], w=[p_])
                    P.op("pe", lambda e, pO=pO, v_=v_, p_=p_, kt=kt: e.matmul(
                        pO[:, :QG], lhsT=r32(v_[:, kt, :]), rhs=r32(p_[:, :]), start=(kt == 0), stop=(kt == NK - 1)), r=[v_, p_], w=[pO])
                    P.op("pe", lambda e, pD=pD, p_=p_, kt=kt: e.matmul(
                        pD[:, :QG], lhsT=r32(ones_r[:, :]), rhs=r32(p_[:, :]), start=(kt == 0), stop=(kt == NK - 1)), r=[ones_r, p_], w=[pD])
                P.op("dve", lambda e, pD=pD: e.reciprocal(out=rd[:, :], in_=pD[:, :QG]), r=[pD], w=[rd])
                P.op("dve", lambda e, pO=pO, o_=o_: e.tensor_tensor(out=o_[:, :], in0=pO[:, :QG], in1=rd[:, :], op=ALU.mult), r=[pO, rd], w=[o_])
                P.dma(lambda e, o_=o_, b=b, hd_=hd_, q0=q0: e.dma_start(out=OT_d[b, hd_ * 128:(hd_ + 1) * 128, q0:q0 + QG], in_=o_[:, :]),
                      r=[o_], w=[OT_d])
            if kv == 1 and hh == 3:
                P.flush("attn")


DEBUG_OUT = set()


def build_program(cfg, nph=99):
    nc = bass.Bass("TRN2", target_bir_lowering=False)
    nc.dge_precook = False
    NB, TB, L, CAP = cfg.NB, cfg.TB, cfg.L, cfg.CAP
    NBc = NB + 1

    def d(name, shape, kind="Internal", dt=F32):
        if name in DEBUG_OUT:
            kind = "ExternalOutput"
        return T(nc.dram_tensor(name, list(shape), dt, kind=kind).ap(), name)

    I = "ExternalInput"
    xc = d("xc", [NB * TB, 1024], I)
    cT = d("cT", [1024, NBc], I)
    modw = d("modw", [2, 1024, 6144], I)
    modb = d("modb", [2, 6144], I)
    norm1 = d("norm1", [2, 1024], I)
    norm2 = d("norm2", [2, 1024], I)
    win = d("win", [1024, WIN_COLS], I)
    convA = d("convA", [512, 3], I)
    convQ = d("convQ", [1536, 3], I)
    gpar = d("gpar", [16, 2], I)
    rm = d("rm", [16, TB], I)
    gn = d("gn", [64], I)
    wout = d("wout", [1024, 1024], I)
    wqkv = d("wqkv", [1024, 1536], I)
    qn = d("qn", [128], I)
    kn = d("kn", [128], I)
    wo = d("wo", [1024, 1024], I)
    cos = d("cos", [L, 64], I)
    sin = d("sin", [L, 64], I)
    wr = d("wr", [2, 1024, 72], I)
    wg = d("wg", [2, 64, 128, 8 * 384], I)
    wu = d("wu", [2, 64, 128, 8 * 384], I)
    wd = d("wd", [2, 64, 128, 3 * 1024], I)
    fnorm = d("fnorm", [1024], I)
    cst = d("cst", [128, C_END], I)
    out = d("out", [NB * L, 1024], "ExternalOutput")
    XR = d("XR", [NB * TB, 1024])
    MOD = d("MOD", [2, NBc, 6144])
    PF = d("PF", [NB, 3072, TB])
    PZ = d("PZ", [NB * TB, 576])
    PG = d("PG", [NB, 64, TB])
    YT = d("YT", [NB, 1024, TB])
    QT = d("QT", [NB, 512, TB])
    KT = d("KT", [NB, 512, TB])
    KTOK = d("KTOK", [NB * TB, 512])
    VTOK = d("VTOK", [NB * TB, 512])
    CB = d("CB", [NB, 32, TB])
    OF = d("OF", [NB * TB, 512])
    OB = d("OB", [NB * TB, 512])
    XB = d("XB", [64 * CAP, 1024])
    YB = d("YB", [64 * CAP, 1024])
    RTI = d("RTI", [NB * TB, 2], dt=I32)
    RTW = d("RTW", [NB * TB, 2])
    QT1 = d("QT1", [NB, 1024, L])
    KT1 = d("KT1", [NB, 256, TB])
    V1 = d("V1", [NB * TB, 256])
    OT = d("OT", [NB, 1024, L])

    def sub(t, idx):
        s = T(t.ap[idx], t.key)
        s.key = t.key
        return s

    with ExitStack() as st:
        P = Prog(nc, st)
        n = NB * TB
        step = max(128, (n // 8) // 128 * 128)
        for r0 in range(0, n, step):
            r1 = min(n, r0 + step)
            P.dma(lambda e, r0=r0, r1=r1: e.dma_start(out=XR[r0:r1, :], in_=xc[r0:r1, :]), r=[xc], w=[XR])
        P.flush("copy")
        phases = [
            lambda: phase_mod(nc, P, cT, modw, modb, MOD, NBc),
            lambda: phase_proj0(nc, P, cfg, XR, MOD, sub(norm1, 0), win, cst, PF, PZ, PG),
            lambda: phase_prep0(nc, P, cfg, cst, PF, PG, convA, convQ, gpar, rm, YT, QT, KT, KTOK, VTOK, CB),
            lambda: phase_gdn(nc, P, cfg, cst, QT, KT, KTOK, VTOK, CB, [OF, OB]),
            lambda: phase_merge0(nc, P, cfg, cst, OF, OB, PZ, gn, YT),
            lambda: phase_outproj(nc, P, cfg, YT, wout, MOD, 0, XR, True),
            lambda: phase_route(nc, P, cfg, cst, XR, MOD, 0, sub(norm2, 0), sub(wr, 0), XB, RTI, RTW, True),
            lambda: phase_experts(nc, P, cfg, cst, XB, sub(wg, 0), sub(wu, 0), sub(wd, 0), YB),
            lambda: phase_combine(nc, P, cfg, XR, MOD, 0, YB, RTI, RTW, True),
            lambda: phase_proj1(nc, P, cfg, cst, XR, MOD, sub(norm1, 1), wqkv, qn, kn, cos, sin, QT1, KT1, V1),
            lambda: phase_attn(nc, P, cfg, cst, QT1, KT1, V1, OT),
            lambda: phase_outproj(nc, P, cfg, OT, wo, MOD, 1, XR, False),
            lambda: phase_route(nc, P, cfg, cst, XR, MOD, 1, sub(norm2, 1), sub(wr, 1), XB, RTI, RTW, False),
            lambda: phase_experts(nc, P, cfg, cst, XB, sub(wg, 1), sub(wu, 1), sub(wd, 1), YB),
            lambda: phase_combine(nc, P, cfg, XR, MOD, 1, YB, RTI, RTW, False, final=(fnorm, out)),
        ]
        for ph in phases[:nph]:
            ph()
        n_inst = P.n_inst
    return nc, n_inst


def host_layout(cfg, inputs, core):
    NB, L, NCTX, TB = cfg.NB, cfg.L, cfg.NCTX, cfg.TB
    f = lambda a: np.ascontiguousarray(np.asarray(a, dtype=np.float32))
    bs = slice(core * NB, (core + 1) * NB)
    x = np.asarray(inputs["x"])[bs]
    ctx = np.asarray(inputs["ctx"])[bs]
    xc = np.concatenate([x, ctx], axis=1).reshape(NB * TB, 1024)
    cT = np.concatenate([np.asarray(inputs["c"])[bs], np.asarray(inputs["c_ctx"])[None, :]], axis=0).T
    w_in = np.asarray(inputs["ab_w_in"])[0]
    gates = w_in[:, 3584:3616]
    z16 = np.zeros((1024, 16), np.float32)
    win = np.concatenate([w_in[:, :3584], gates[:, 0:8], gates[:, 16:24], z16, gates[:, 8:16], gates[:, 24:32], z16], axis=1)
    a_log = np.asarray(inputs["ab_a_log"])[0].reshape(16)
    dtb = np.asarray(inputs["ab_dt_bias"])[0].reshape(16)
    rm = np.ones((16, TB), np.float32)
    rm[:, ::64] = 0
    n_freq = 32
    inv = (10000.0 ** (-np.arange(n_freq, dtype=np.float32) / n_freq)).astype(np.float32)
    t = np.arange(L)
    ang = np.concatenate([(t // cfg.GRID_W).astype(np.float32)[:, None] * inv, (t % cfg.GRID_W).astype(np.float32)[:, None] * inv], axis=-1)
    m = {
        "xc": f(xc), "cT": f(cT), "modw": f(inputs["mod_w"]), "modb": f(inputs["mod_b"]),
        "norm1": f(inputs["norm1"]), "norm2": f(inputs["norm2"]), "win": f(win),
        "convA": f(np.asarray(inputs["ab_conv_a"])[0].T), "convQ": f(np.asarray(inputs["ab_conv_qkv"])[0].T),
        "gpar": f(np.stack([dtb, a_log], axis=1)), "rm": rm, "gn": f(np.asarray(inputs["ab_gnorm"])[0]),
        "wout": f(np.asarray(inputs["ab_w_out"])[0]), "wqkv": f(np.asarray(inputs["attn_w_qkv"])[0]),
        "qn": f(np.asarray(inputs["attn_q_norm"])[0]), "kn": f(np.asarray(inputs["attn_k_norm"])[0]),
        "wo": f(np.asarray(inputs["attn_w_o"])[0]), "cos": f(np.cos(ang)), "sin": f(np.sin(ang)),
        "wr": f(np.concatenate([np.asarray(inputs["moe_w_group"]), np.asarray(inputs["moe_w_expert"])], axis=2)),
        "wg": f(np.asarray(inputs["moe_w_gate"]).reshape(2, 64, 8, 128, 384).transpose(0, 1, 3, 2, 4).reshape(2, 64, 128, 8 * 384)),
        "wu": f(np.asarray(inputs["moe_w_up"]).reshape(2, 64, 8, 128, 384).transpose(0, 1, 3, 2, 4).reshape(2, 64, 128, 8 * 384)),
        "wd": f(np.asarray(inputs["moe_w_down"]).reshape(2, 64, 3, 128, 1024).transpose(0, 1, 3, 2, 4).reshape(2, 64, 128, 3 * 1024)),
        "fnorm": f(inputs["final_norm"]), "cst": build_consts(),
    }
    return m


def kernel(**inputs):
    n_cores = 8
    cfg = Cfg(NB=4, L=2048, NCTX=256, GRID_W=64, CAP=512)
    nc, _ = build_program(cfg)
    shared = None
    in_maps = []
    for c in range(n_cores):
        m = host_layout(cfg, inputs, c) if shared is None else None
        if shared is None:
            shared = m
        else:
            m = dict(shared)
            bs = slice(c * cfg.NB, (c + 1) * cfg.NB)
            x = np.asarray(inputs["x"])[bs]
            ctx = np.asarray(inputs["ctx"])[bs]
            m["xc"] = np.ascontiguousarray(np.concatenate([x, ctx], axis=1).reshape(cfg.NB * cfg.TB, 1024), dtype=np.float32)
            m["cT"] = np.ascontiguousarray(np.concatenate([np.asarray(inputs["c"])[bs], np.asarray(inputs["c_ctx"])[None, :]], axis=0).T, dtype=np.float32)
        in_maps.append(m)
    res = run_bass_kernel_spmd(nc, in_maps, core_ids=list(range(n_cores)))
    outs = [np.asarray(r["out"]).reshape(cfg.NB, cfg.L, 1024) for r in res.results]
    return np.concatenate(outs, axis=0).astype(np.float32)
```
